# Optimizing a Trainium2 kernel written in Bass

```python
import math
import jax, jax.numpy as jnp
from jax import lax
import numpy as np

D_MODEL = 1024
BATCH = 2
SEQ = 8192
DEPTH = 1

GRID_W = 64
CTX_LEN = 256

N_HEADS = 8
Q_LORA = 512
KV_LORA = 256
QK_NOPE = 64
QK_ROPE = 32
V_HEAD = 64
ROPE_FREQS = QK_ROPE // 4
ROPE_THETA = 10000.0
ATTN_SCALE = 1.0 / math.sqrt(QK_NOPE + QK_ROPE)
ATTN_WIDTH = N_HEADS * V_HEAD
Q_BLOCK = 128

POOL_GROUPS = 4
POOL_WINDOWS = (2, 4, 8, 16)
POOL_WIDTH = D_MODEL // 2
POOL_GC = POOL_WIDTH // POOL_GROUPS

MIX_WIDTH = ATTN_WIDTH + POOL_WIDTH
IN_WIDTH = Q_LORA + KV_LORA + QK_ROPE + POOL_WIDTH

N_EXPERTS = 64
N_EXPERT_GROUPS = 8
TOPK_GROUPS = 4
TOP_K = 8
D_EXPERT = 256
D_SHARED = 256
ROUTED_SCALE = 2.5
MOE_BLOCK = 128

LN_EPS = 1e-5
RMS_EPS = 1e-6

kernel_name = "hybrid_mla_pool_moe_flow_block"


def layer_norm(x, g, b):
    xf = x.astype(jnp.float32)
    mu = xf.mean(-1, keepdims=True)
    var = jnp.square(xf - mu).mean(-1, keepdims=True)
    y = (xf - mu) * lax.rsqrt(var + LN_EPS)
    return (y * g.astype(jnp.float32) + b.astype(jnp.float32)).astype(x.dtype)


def rms_norm(x, g):
    xf = x.astype(jnp.float32)
    y = xf * lax.rsqrt(jnp.square(xf).mean(-1, keepdims=True) + RMS_EPS)
    return (y * g.astype(jnp.float32)).astype(x.dtype)


def axial_rope_tables(rows, dtype):
    t = jnp.arange(rows * GRID_W)
    pos = jnp.stack([t // GRID_W, t % GRID_W], axis=-1).astype(jnp.float32)
    inv_freq = ROPE_THETA ** (-jnp.arange(ROPE_FREQS, dtype=jnp.float32) / ROPE_FREQS)
    ang = pos[:, :, None] * inv_freq
    return (jnp.cos(ang)[:, :, None, :].astype(dtype),
            jnp.sin(ang)[:, :, None, :].astype(dtype))


def apply_axial_rope(x, cos, sin):
    xs = x.reshape(x.shape[:-1] + (2, 2, ROPE_FREQS))
    rot = jnp.concatenate([-xs[..., 1:, :], xs[..., :1, :]], axis=-2)
    return (xs * cos + rot * sin).reshape(x.shape)


def mla_keys(p, kv_norm_g, w_ukv, cos, sin):
    b_, n = p.shape[:2]
    kv_lat = p[..., Q_LORA:Q_LORA + KV_LORA]
    k_rope = p[..., Q_LORA + KV_LORA:Q_LORA + KV_LORA + QK_ROPE]
    kv = (rms_norm(kv_lat, kv_norm_g) @ w_ukv).reshape(b_, n, N_HEADS, QK_NOPE + V_HEAD)
    k_nope, v = kv[..., :QK_NOPE], kv[..., QK_NOPE:]
    if cos is not None:
        k_rope = apply_axial_rope(k_rope, cos, sin)
    return k_nope, k_rope, v


def mla_queries(p, q_norm_g, w_uq, cos, sin):
    b_, n = p.shape[:2]
    q = (rms_norm(p[..., :Q_LORA], q_norm_g) @ w_uq).reshape(b_, n, N_HEADS, QK_NOPE + QK_ROPE)
    q_nope, q_rope = q[..., :QK_NOPE], q[..., QK_NOPE:]
    if cos is not None:
        q_rope = apply_axial_rope(q_rope, cos[:, None], sin[:, None])
    return q_nope, q_rope


def attend(q_nope, q_rope, k_nope, k_rope, v):
    s = (jnp.einsum('bqhn,bkhn->bhqk', q_nope, k_nope)
         + jnp.einsum('bqhr,bkr->bhqk', q_rope, k_rope)).astype(jnp.float32) * ATTN_SCALE
    prob = jax.nn.softmax(s, axis=-1).astype(v.dtype)
    return jnp.einsum('bhqk,bkhv->bqhv', prob, v)


def latent_attention(q_nope, q_rope, k_nope, k_rope, v):
    b_, s = q_nope.shape[:2]
    nb = s // Q_BLOCK

    def to_blocks(a):
        return a.reshape((b_, nb, Q_BLOCK) + a.shape[2:]).swapaxes(0, 1)

    out = lax.map(lambda qs: attend(qs[0], qs[1], k_nope, k_rope, v),
                  (to_blocks(q_nope), to_blocks(q_rope)))
    return out.swapaxes(0, 1).reshape(b_, s, ATTN_WIDTH)


def multiscale_pool(u, w_pool, pool_scale):
    b_, n = u.shape[:2]
    ug = u.reshape(b_, n, POOL_GROUPS, POOL_GC)
    cs = jnp.concatenate([jnp.zeros((b_, 1, POOL_GROUPS, POOL_GC), jnp.float32),
                          jnp.cumsum(ug.astype(jnp.float32), axis=1)], axis=1)
    t = jnp.arange(n)
    means = []
    for g, w in enumerate(POOL_WINDOWS):
        lo = jnp.clip(t - w // 2, 0, n)
        hi = jnp.clip(t - w // 2 + w, 0, n)
        csg = cs[:, :, g]
        means.append((csg[:, hi] - csg[:, lo]) / (hi - lo).astype(jnp.float32)[:, None])
    pooled = jnp.stack(means, axis=2).astype(u.dtype) - ug
    out = jnp.einsum('bngc,gcd->bngd', pooled, w_pool).reshape(b_, n, POOL_WIDTH)
    return out * pool_scale


def moe_ffn(h, w_router, router_bias, w_e_gate, w_e_up, w_e_down, w_s_gate, w_s_up, w_s_down):
    lead = h.shape[:-1]
    hf = h.reshape(-1, D_MODEL)
    n = hf.shape[0]
    scores = jax.nn.sigmoid((hf @ w_router).astype(jnp.float32))
    biased = scores + router_bias.astype(jnp.float32)
    grp = biased.reshape(n, N_EXPERT_GROUPS, N_EXPERTS // N_EXPERT_GROUPS)
    grp_score = lax.top_k(grp, 2)[0].sum(-1)
    _, top_groups = lax.top_k(grp_score, TOPK_GROUPS)
    group_mask = jax.nn.one_hot(top_groups, N_EXPERT_GROUPS, dtype=jnp.float32).sum(-2)
    expert_mask = jnp.repeat(group_mask, N_EXPERTS // N_EXPERT_GROUPS, axis=-1)
    masked = jnp.where(expert_mask > 0, biased, -jnp.inf)
    _, idx = lax.top_k(masked, TOP_K)
    wsel = jnp.take_along_axis(scores, idx, axis=-1)
    wsel = wsel / wsel.sum(-1, keepdims=True) * ROUTED_SCALE
    gates = (jax.nn.one_hot(idx, N_EXPERTS, dtype=jnp.float32) * wsel[..., None]).sum(-2).astype(h.dtype)

    def expert_block(args):
        hb, gb = args
        a = jax.nn.silu(jnp.einsum('td,edf->tef', hb, w_e_gate)) * jnp.einsum('td,edf->tef', hb, w_e_up)
        return jnp.einsum('tef,efd->td', a * gb[:, :, None], w_e_down)

    nb = n // MOE_BLOCK
    routed = lax.map(expert_block, (hf.reshape(nb, MOE_BLOCK, D_MODEL),
                                    gates.reshape(nb, MOE_BLOCK, N_EXPERTS))).reshape(n, D_MODEL)
    shared = (jax.nn.silu(hf @ w_s_gate) * (hf @ w_s_up)) @ w_s_down
    return (routed + shared).reshape(lead + (D_MODEL,))


def setup_inputs(seed: int = 0) -> dict:
    key = jax.random.key(seed)
    ks = jax.random.split(key, 32)
    L = DEPTH
    beta = (8.0 * DEPTH) ** -0.25

    def nrm(k, shape, scale):
        return jax.random.normal(k, shape, jnp.float32) * scale

    return {
        "x": nrm(ks[0], (BATCH, SEQ, D_MODEL), 1.0),
        "c": nrm(ks[1], (BATCH, D_MODEL), 1.0),
        "ctx": nrm(ks[2], (BATCH, CTX_LEN, D_MODEL), 1.0),
        "c_ctx": nrm(ks[3], (D_MODEL,), 1.0),
        "w_ada": nrm(ks[4], (L, D_MODEL, 6 * D_MODEL), 0.5 * D_MODEL ** -0.5),
        "b_ada": nrm(ks[5], (L, 6 * D_MODEL), 0.02),
        "w_in": nrm(ks[6], (L, D_MODEL, IN_WIDTH), D_MODEL ** -0.5),
        "q_norm_g": 1.0 + nrm(ks[7], (L, Q_LORA), 0.02),
        "w_uq": nrm(ks[8], (L, Q_LORA, N_HEADS * (QK_NOPE + QK_ROPE)), Q_LORA ** -0.5),
        "kv_norm_g": 1.0 + nrm(ks[9], (L, KV_LORA), 0.02),
        "w_ukv": nrm(ks[10], (L, KV_LORA, N_HEADS * (QK_NOPE + V_HEAD)), KV_LORA ** -0.5),
        "w_pool": nrm(ks[11], (L, POOL_GROUPS, POOL_GC, POOL_GC), POOL_GC ** -0.5),
        "pool_scale": 1.0 + nrm(ks[12], (L, POOL_WIDTH), 0.02),
        "w_out": nrm(ks[13], (L, MIX_WIDTH, D_MODEL), beta * MIX_WIDTH ** -0.5),
        "ln1_g": 1.0 + nrm(ks[14], (L, D_MODEL), 0.02),
        "ln1_b": nrm(ks[15], (L, D_MODEL), 0.02),
        "w_router": nrm(ks[16], (L, D_MODEL, N_EXPERTS), D_MODEL ** -0.5),
        "router_bias": nrm(ks[17], (L, N_EXPERTS), 0.01),
        "w_e_gate": nrm(ks[18], (L, N_EXPERTS, D_MODEL, D_EXPERT), D_MODEL ** -0.5),
        "w_e_up": nrm(ks[19], (L, N_EXPERTS, D_MODEL, D_EXPERT), D_MODEL ** -0.5),
        "w_e_down": nrm(ks[20], (L, N_EXPERTS, D_EXPERT, D_MODEL), beta * D_EXPERT ** -0.5),
        "w_s_gate": nrm(ks[21], (L, D_MODEL, D_SHARED), D_MODEL ** -0.5),
        "w_s_up": nrm(ks[22], (L, D_MODEL, D_SHARED), D_MODEL ** -0.5),
        "w_s_down": nrm(ks[23], (L, D_SHARED, D_MODEL), beta * D_SHARED ** -0.5),
        "ln2_g": 1.0 + nrm(ks[24], (L, D_MODEL), 0.02),
        "ln2_b": nrm(ks[25], (L, D_MODEL), 0.02),
    }


def reference(x, c, ctx, c_ctx, w_ada, b_ada, w_in, q_norm_g, w_uq, kv_norm_g, w_ukv,
              w_pool, pool_scale, w_out, ln1_g, ln1_b, w_router, router_bias,
              w_e_gate, w_e_up, w_e_down, w_s_gate, w_s_up, w_s_down, ln2_g, ln2_b):
    b_, s = x.shape[:2]
    rows = s // GRID_W
    cos, sin = axial_rope_tables(rows, x.dtype)
    alpha = (2.0 * DEPTH) ** 0.25
    silu_c = jax.nn.silu(c)
    silu_cc = jax.nn.silu(c_ctx)

    for l in range(DEPTH):
        last = l == DEPTH - 1
        moe_args = (w_router[l], router_bias[l], w_e_gate[l], w_e_up[l], w_e_down[l],
                    w_s_gate[l], w_s_up[l], w_s_down[l])
        sh1, sc1, g1, sh2, sc2, g2 = jnp.split(silu_c @ w_ada[l] + b_ada[l], 6, axis=-1)
        sh1c, sc1c, g1c, sh2c, sc2c, g2c = jnp.split(silu_cc @ w_ada[l] + b_ada[l], 6, axis=-1)

        h = x * (1 + sc1[:, None]) + sh1[:, None]
        hc = ctx * (1 + sc1c) + sh1c
        p = h @ w_in[l]
        pc = hc @ w_in[l]

        q_nope, q_rope = mla_queries(p, q_norm_g[l], w_uq[l], cos, sin)
        k_nope, k_rope, v = mla_keys(p, kv_norm_g[l], w_ukv[l], cos, sin)
        kc_nope, kc_rope, vc = mla_keys(pc, kv_norm_g[l], w_ukv[l], None, None)
        attn = latent_attention(q_nope, q_rope,
                                jnp.concatenate([kc_nope, k_nope], axis=1),
                                jnp.concatenate([kc_rope, k_rope], axis=1),
                                jnp.concatenate([vc, v], axis=1))
        pooled = multiscale_pool(p[..., IN_WIDTH - POOL_WIDTH:], w_pool[l], pool_scale[l])

        y = jnp.concatenate([attn, pooled], axis=-1) @ w_out[l]
        x1 = layer_norm(alpha * x + g1[:, None] * y, ln1_g[l], ln1_b[l])
        h2 = x1 * (1 + sc2[:, None]) + sh2[:, None]
        x = layer_norm(alpha * x1 + g2[:, None] * moe_ffn(h2, *moe_args), ln2_g[l], ln2_b[l])

        if not last:
            qc_nope, qc_rope = mla_queries(pc, q_norm_g[l], w_uq[l], None, None)
            attn_c = attend(qc_nope, qc_rope, kc_nope, kc_rope, vc).reshape(b_, CTX_LEN, ATTN_WIDTH)
            pooled_c = multiscale_pool(pc[..., IN_WIDTH - POOL_WIDTH:], w_pool[l], pool_scale[l])
            yc = jnp.concatenate([attn_c, pooled_c], axis=-1) @ w_out[l]
            ctx1 = layer_norm(alpha * ctx + g1c * yc, ln1_g[l], ln1_b[l])
            hc2 = ctx1 * (1 + sc2c) + sh2c
            ctx = layer_norm(alpha * ctx1 + g2c * moe_ffn(hc2, *moe_args), ln2_g[l], ln2_b[l])

    return x
```

```python
import math
from contextlib import ExitStack

import numpy as np
import concourse.bass as bass
import concourse.mybir as mybir
from concourse.bass_utils import run_bass_kernel_spmd

F32 = mybir.dt.float32
BF16 = mybir.dt.bfloat16
AF = mybir.ActivationFunctionType
ALU = mybir.AluOpType
AX = mybir.AxisListType

D = 1024
SEQ = 8192
NCORE = 8
TOWN = 2048
CTX = 256
NKEY = SEQ + CTX
NH = 8
ATTN_SCALE = 1.0 / math.sqrt(96.0)
ALPHA = 2.0 ** 0.25
LN_EPS = 1e-5
RMS_EPS = 1e-6
NEXP = 64
SBUF_BASE = 16512
SBUF_LIMIT = 229376


class Prog:
    ENGS = ("pe", "act", "dve", "pool", "sp")

    def __init__(self):
        self.ops = []
        self.res_w = {}
        self.res_r = {}
        self.last_eng = {}
        self.last_chan = {}

    def op(self, eng, fn, reads=(), writes=(), dma=False, extra_deps=()):
        idx = len(self.ops)
        deps = set(extra_deps)
        for r in reads:
            w = self.res_w.get(r)
            if w is not None:
                deps.add(w)
        for k in writes:
            w = self.res_w.get(k)
            if w is not None:
                deps.add(w)
            for rd in self.res_r.get(k, ()):
                deps.add(rd)
        for r in reads:
            self.res_r.setdefault(r, []).append(idx)
        for k in writes:
            self.res_w[k] = idx
            self.res_r[k] = []
        deps.discard(idx)
        chan = writes[0] if dma else None
        self.ops.append(dict(eng=eng, fn=fn, deps=deps, dma=dma, chan=chan))
        if dma:
            self.last_chan[chan] = idx
        elif fn is not None:
            self.last_eng[eng] = idx
        return idx

    def barrier(self):
        deps = list(self.last_eng.values()) + list(self.last_chan.values())
        for e in self.ENGS:
            self.op(e, None, extra_deps=deps)
        self.res_w.clear()
        self.res_r.clear()

    def finalize(self, nc, stack):
        ops = self.ops

        def skip(p, o):
            return (not p["dma"]) and (not o["dma"]) and p["eng"] == "pe" and o["eng"] == "pe" \
                and p["fn"] is not None and o["fn"] is not None

        need = [False] * len(ops)
        for o in ops:
            for d in o["deps"]:
                p = ops[d]
                if p["dma"] or skip(p, o):
                    continue
                need[d] = True
        self.esem = {e: stack.enter_context(nc.semaphore("s_" + e)) for e in self.ENGS}
        chans = []
        seen = set()
        for o in ops:
            if o["dma"] and o["chan"] not in seen:
                seen.add(o["chan"])
                chans.append(o["chan"])
        self.csem = {c: stack.enter_context(nc.semaphore("d%d" % i)) for i, c in enumerate(chans)}
        ecount = {e: 0 for e in self.ENGS}
        ccount = {c: 0 for c in chans}
        for i, o in enumerate(ops):
            if o["dma"]:
                ccount[o["chan"]] += 16
                o["sig"] = (self.csem[o["chan"]], 16, ccount[o["chan"]])
            elif need[i]:
                ecount[o["eng"]] += 1
                o["sig"] = (self.esem[o["eng"]], 1, ecount[o["eng"]])
            else:
                o["sig"] = None
        waited = {e: {} for e in self.ENGS}
        for o in ops:
            ws = {}
            for d in o["deps"]:
                p = ops[d]
                if skip(p, o):
                    continue
                sem, _, val = p["sig"]
                key = id(sem)
                if key not in ws or ws[key][1] < val:
                    ws[key] = (sem, val)
            out = []
            for key, (sem, val) in ws.items():
                if waited[o["eng"]].get(key, 0) >= val:
                    continue
                waited[o["eng"]][key] = val
                out.append((sem, val))
            o["waits"] = out
        self.nsem = len(chans) + len(self.ENGS)

    def emit(self, ename, eng):
        for o in self.ops:
            if o["eng"] != ename:
                continue
            for sem, val in o["waits"]:
                eng.wait_ge(sem, val)
            if o["fn"] is None:
                continue
            ins = o["fn"](eng)
            if o["sig"] is not None:
                ins.then_inc(o["sig"][0], o["sig"][1])


class Plan:
    def __init__(self):
        self.items = []

    def add(self, name, shape, dtype, p0, p1):
        nbytes = int(np.prod(shape[1:])) * (4 if dtype == F32 else 2)
        nbytes = (nbytes + 63) // 64 * 64
        self.items.append(dict(name=name, shape=list(shape), dtype=dtype, p0=p0, p1=p1, n=nbytes))

    def place(self, nc):
        for ph in sorted(set([i["p0"] for i in self.items] + [i["p1"] for i in self.items])):
            tot = sum(i["n"] for i in self.items if i["p0"] <= ph <= i["p1"])
            print("phase", ph, "sbuf bytes", tot)
        placed = []
        out = {}
        for it in sorted(self.items, key=lambda t: -t["n"]):
            conf = sorted([(q["off"], q["off"] + q["n"]) for q in placed
                           if not (q["p1"] < it["p0"] or it["p1"] < q["p0"])])
            off = SBUF_BASE
            for a, b in conf:
                if off + it["n"] <= a:
                    break
                off = max(off, b)
            it["off"] = off
            assert off + it["n"] <= SBUF_LIMIT, ("SBUF overflow", it["name"], off + it["n"])
            placed.append(it)
            out[it["name"]] = nc.alloc_sbuf_tensor_at(it["name"], it["shape"], it["dtype"], offset=off)
        return out


def bc_last(ap2d, n):
    a = ap2d.ap
    return bass.AP(ap2d.tensor, ap2d.offset, [list(a[0]), list(a[1]), [0, n]])


def bc_mid(ap2d, n):
    a = ap2d.ap
    return bass.AP(ap2d.tensor, ap2d.offset, [list(a[0]), [0, n], list(a[1])])


def build_program(debug=None):
    nc = bass.Bass("TRN2", target_bir_lowering=False)
    P = Prog()

    def din(name, shape, dt=F32):
        return nc.dram_tensor(name, list(shape), dt, kind="ExternalInput").ap()

    xk = din("xk", [NKEY, D])
    xhalo = din("xhalo", [4, 16, D])
    hmask = din("hmask", [128, 4, 16])
    icntc = din("icntc", [16, 512])
    cosk = din("cosk", [32, NKEY])
    sink = din("sink", [32, NKEY])
    cvec = din("cvec", [128, 8, 2])
    ident_d = din("ident", [128, 128])
    w_ada = din("w_ada", [D, 6 * D])
    b_ada_fm = din("b_ada_fm", [128, 48])
    b_ada_row = din("b_ada_row", [1, 6 * D])
    w_in = din("w_in", [D, 1312])
    qg_fm = din("qg_fm", [128, 4])
    kvg_fm = din("kvg_fm", [128, 2])
    w_uq = din("w_uq", [512, 768])
    w_ukv = din("w_ukv", [256, 1024])
    w_pool = din("w_pool", [4, 128, 128])
    pscale_fm = din("pscale_fm", [128, 4])
    w_out = din("w_out", [D, D])
    ln1_g = din("ln1_g", [1, D])
    ln1_b = din("ln1_b", [1, D])
    ln2_g = din("ln2_g", [1, D])
    ln2_b = din("ln2_b", [1, D])
    w_router = din("w_router", [D, NEXP])
    rbias = din("rbias", [1, NEXP])
    w_eg = din("w_eg", [NEXP + 1, D, 256])
    w_eu = din("w_eu", [NEXP + 1, D, 256])
    w_ed = din("w_ed", [NEXP + 1, 256, D])
    yout = nc.dram_tensor("yout", [TOWN, D], F32, kind="ExternalOutput").ap()
    dbg_out = {}
    if debug:
        for name, shape in debug.items():
            dbg_out[name] = nc.dram_tensor("dbg_" + name, list(shape), F32, kind="ExternalOutput").ap()

    pl = Plan()
    A = pl.add
    A("ident", [128, 128], F32, 0, 6)
    A("onesb", [128, 128], BF16, 0, 6)
    A("modT", [128, 96], F32, 0, 6)
    A("mods", [128, 64], F32, 0, 6)
    A("g1bc", [128, D], F32, 0, 3)
    A("g2bc", [128, D], F32, 0, 5)
    A("stat", [128, 16, 8], F32, 0, 6)
    A("wada0", [128, 8, 512], F32, 0, 0)
    A("wada1", [128, 8, 512], F32, 0, 0)
    A("cT", [128, 8, 2], F32, 0, 0)
    A("scT", [128, 8, 2], F32, 0, 0)
    A("modrow", [2, 6 * D], F32, 0, 0)
    A("brow", [2, 6 * D], F32, 0, 0)
    A("sel2", [2, 128], F32, 0, 0)
    A("kvnT", [128, 2, NKEY], BF16, 0.5, 2)
    A("KT", [128, NKEY], BF16, 1, 2)
    A("qnT", [128, 4, TOWN], BF16, 1, 2)
    A("pooledT", [128, 4, TOWN], BF16, 1, 3)
    A("WUKV", [128, 2, 1024], BF16, 0.5, 2)
    A("WUQ", [128, 4, 768], BF16, 0.5, 2)
    A("WUQR", [128, 4, 256], BF16, 0.5, 2)
    A("WIN", [128, 8, 1312], BF16, 0.5, 1)
    A("WKR32", [128, 8, 32], F32, 0.5, 0.5)
    A("WKR", [128, 8, 32], BF16, 0.5, 1)
    A("WPOOL", [128, 4, 128], BF16, 0.5, 1)
    A("stg32", [128, 4, 768], F32, 0.5, 0.5)
    A("qg", [128, 4], F32, 0.5, 0.5)
    A("kvg", [128, 2], F32, 0.5, 0.5)
    A("psc", [128, 4], F32, 0.5, 1)
    for i in range(4):
        A("xt%d" % i, [128, D], F32, 1, 1)
    A("xh", [16, D], F32, 1, 1)
    A("hT0", [128, 8, 512], BF16, 1, 1)
    A("hT1", [128, 8, 512], BF16, 1, 1)
    A("hTh", [128, 8, 16], BF16, 1, 1)
    A("sq", [128, 4, 512], BF16, 1, 1)
    A("rbc", [128, 512], F32, 1, 1)
    A("latsb", [128, 2, 512], F32, 1, 1)
    A("cs0", [128, 2, 512], F32, 1, 2)
    A("cs1", [128, 2, 512], F32, 1, 2)
    A("rt1", [128, 512], F32, 1, 2)
    A("rt2", [128, 512], F32, 1, 2)
    A("uext", [128, 4, 528], F32, 1, 1)
    A("ic0", [128, 512], F32, 1, 1)
    A("pa", [128, 528], F32, 1, 1)
    A("pb", [128, 528], F32, 1, 1)
    A("ic1", [128, 512], F32, 1, 1)
    A("pin", [128, 4, 512], BF16, 1, 1)
    A("hm", [128, 4, 16], F32, 0.5, 1)
    A("attnT", [64, 8, TOWN], BF16, 2, 3)
    A("VH0", [128, 66, 65], BF16, 2, 2)
    A("VH1", [128, 66, 65], BF16, 2, 2)
    A("QH", [128, TOWN], BF16, 2, 2)
    for i in range(3):
        A("PTP%d" % i, [128, 1024], BF16, 2, 2)
    A("rden", [128, 512], F32, 2, 2)
    A("num", [64, 512], F32, 2, 2)
    A("c1row", [128, 64], F32, 2, 2)
    A("WOA", [64, 8, D], BF16, 3, 3)
    A("WOP", [128, 4, D], BF16, 3, 3)
    A("acc", [128, 16, D], F32, 3, 6)
    for i in range(4):
        A("xs%d" % i, [128, D], F32, 3, 3)
    A("l1g", [128, D], F32, 3, 3)
    A("l1b", [128, D], F32, 3, 3)
    A("h2T", [128, 8, TOWN], BF16, 4, 5)
    A("h2f0", [128, 8, 128], F32, 4, 4)
    A("h2f1", [128, 8, 128], F32, 4, 4)
    A("wr", [128, 8, NEXP], F32, 4, 4)
    A("rb", [128, NEXP], F32, 4, 4)
    A("scores", [128, 16, NEXP], F32, 4, 4)
    A("gates", [128, 16, NEXP], F32, 4, 5)
    A("ra", [128, 16, NEXP], F32, 4, 4)
    A("rbb", [128, 16, NEXP], F32, 4, 4)
    A("rc", [128, 16, NEXP], F32, 4, 4)
    A("rs", [128, 16, 8], F32, 4, 4)
    A("rs2", [128, 16, 8], F32, 4, 4)
    A("rs3", [128, 16, 8], F32, 4, 4)
    A("rs4", [128, 16, 8], F32, 4, 4)
    A("t8", [128, 16, 8], F32, 4, 4)
    A("rsum", [128, 16], F32, 4, 4)
    for i in range(2):
        A("wg%d" % i, [128, 8, 256], BF16, 4, 5)
        A("wu%d" % i, [128, 8, 256], BF16, 4, 5)
        A("wd32_%d" % i, [128, 2, D], F32, 4, 5)
        A("wd%d" % i, [128, 2, D], BF16, 4, 5)
        A("sg%d" % i, [128, 512], F32, 5, 5)
        A("aT%d" % i, [128, 2, 512], BF16, 5, 5)
    A("l2g", [128, D], F32, 5, 6)
    A("l2b", [128, D], F32, 5, 6)
    for i in range(4):
        A("o%d" % i, [128, D], F32, 5, 6)
    T = pl.place(nc)

    st = ExitStack()
    PSP = [st.enter_context(nc.psum_tensor("psp%d" % i, [128, 1024], F32)) for i in range(4)]
    PS = [PSP[i // 2][:, (i % 2) * 512:(i % 2 + 1) * 512] for i in range(8)]

    def B(i):
        return ("B", i)

    def dma(q, out, in_, reads=(), writes=(), nonc=False):
        if nonc:
            def fn(e):
                with nc.allow_non_contiguous_dma(reason="small strided constant load"):
                    return e.dma_start(out=out, in_=in_)
        else:
            def fn(e):
                return e.dma_start(out=out, in_=in_)
        return P.op(q, fn, reads=list(reads), writes=list(writes), dma=True)

    def ln_tile(src_ap, dst_ap, gbc, bbc, skey, slot, rsrc, wdst, extra_reads=()):
        s = T["stat"][:, slot, :]
        skey = (skey, slot)
        P.op("act", lambda e: e.activation(out=dst_ap, in_=src_ap, func=AF.Identity, accum_out=s[:, 0:1]),
             reads=[rsrc, "statz"] + list(extra_reads), writes=[wdst, (skey, 0)])
        P.op("act", lambda e: e.activation(out=dst_ap, in_=src_ap, func=AF.Square, accum_out=s[:, 1:2]),
             reads=[rsrc, "statz"], writes=[wdst, (skey, 1)])
        P.op("dve", lambda e: e.tensor_scalar(out=s[:, 2:3], in0=s[:, 0:1], scalar1=1.0 / D, scalar2=None, op0=ALU.mult),
             reads=[(skey, 0)], writes=[(skey, 2)])
        P.op("dve", lambda e: e.tensor_tensor(out=s[:, 3:4], in0=s[:, 2:3], in1=s[:, 2:3], op=ALU.mult),
             reads=[(skey, 2)], writes=[(skey, 3)])
        P.op("dve", lambda e: e.scalar_tensor_tensor(out=s[:, 4:5], in0=s[:, 1:2], scalar=1.0 / D, in1=s[:, 3:4],
                                                     op0=ALU.mult, op1=ALU.subtract),
             reads=[(skey, 1), (skey, 3)], writes=[(skey, 4)])
        P.op("dve", lambda e: e.tensor_scalar(out=s[:, 4:5], in0=s[:, 4:5], scalar1=LN_EPS, scalar2=None, op0=ALU.add),
             reads=[(skey, 4)], writes=[(skey, 4)])
        P.op("act", lambda e: e.activation(out=s[:, 7:8], in_=s[:, 4:5], func=AF.Sqrt),
             reads=[(skey, 4)], writes=[(skey, 7)])
        P.op("dve", lambda e: e.reciprocal(out=s[:, 5:6], in_=s[:, 7:8]),
             reads=[(skey, 7)], writes=[(skey, 5)])
        P.op("dve", lambda e: e.scalar_tensor_tensor(out=s[:, 6:7], in0=s[:, 2:3], scalar=-1.0, in1=s[:, 5:6],
                                                     op0=ALU.mult, op1=ALU.mult),
             reads=[(skey, 2), (skey, 5)], writes=[(skey, 6)])
        P.op("act", lambda e: e.activation(out=dst_ap, in_=src_ap, func=AF.Identity, bias=s[:, 6:7], scale=s[:, 5:6]),
             reads=[rsrc, (skey, 5), (skey, 6), (skey, 1)], writes=[wdst])
        P.op("dve", lambda e: e.tensor_tensor(out=dst_ap, in0=dst_ap, in1=gbc, op=ALU.mult),
             reads=[wdst], writes=[wdst])
        P.op("dve", lambda e: e.tensor_tensor(out=dst_ap, in0=dst_ap, in1=bbc, op=ALU.add),
             reads=[wdst], writes=[wdst])

    def dump(name, ap_sb, reads):
        if debug and name in debug:
            dma("pool", dbg_out[name], ap_sb, reads=reads, writes=[("dbg", name)])

    idn, onesb, modT, mods = T["ident"], T["onesb"], T["modT"], T["mods"]
    modrow, sel2 = T["modrow"], T["sel2"]
    dma("sp", idn[:], ident_d, writes=["ident"])
    dma("sp", T["cT"][:], cvec, writes=["cT"])
    dma("sp", T["brow"][:], b_ada_row.partition_broadcast(2), writes=["brow"])
    P.op("pool", lambda e: e.memset(onesb[:], 1.0), writes=["onesb"])
    P.op("pool", lambda e: e.memset(sel2[:], 0.0), writes=["sel2"])
    P.op("pool", lambda e: e.memset(sel2[0:1, :], 1.0), reads=["sel2"], writes=["sel2"])
    P.op("act", lambda e: e.activation(out=T["scT"][:], in_=T["cT"][:], func=AF.Silu), reads=["cT"], writes=["scT"])
    wada_v = w_ada.rearrange("(j p) n -> p j n", p=128)
    for cb in range(12):
        wb = T["wada%d" % (cb % 2)]
        wk = "wada%d" % (cb % 2)
        dma("sp" if cb % 2 == 0 else "act", wb[:], wada_v[:, :, cb * 512:(cb + 1) * 512], writes=[wk])
        bank = cb % 2

        def fn(e, wb=wb, bank=bank):
            for j in range(8):
                ins = e.matmul(PS[bank][0:2, :], lhsT=T["scT"][:, j, :], rhs=wb[:, j, :], start=(j == 0), stop=(j == 7))
            return ins
        P.op("pe", fn, reads=[wk, "scT"], writes=[B(bank)])
        csl = slice(cb * 512, (cb + 1) * 512)
        P.op("dve", lambda e, bank=bank, csl=csl: e.tensor_tensor(out=modrow[:, csl], in0=PS[bank][0:2, :], in1=T["brow"][:, csl], op=ALU.add),
             reads=[B(bank), "brow"], writes=[("modrow", cb)])
    def fnT(e):
        for ch in range(48):
            ins = e.transpose(out=PS[2][:, ch * 2:ch * 2 + 2], in_=modrow[:, ch * 128:(ch + 1) * 128], identity=idn[0:2, 0:2])
        return ins
    P.op("pe", fnT, reads=["ident"] + [("modrow", cb) for cb in range(12)], writes=[B(2)])
    P.op("dve", lambda e: e.tensor_copy(out=modT[:], in_=PS[2][:, 0:96]), reads=[B(2)], writes=["modT"])
    for gi, (gname, c0) in enumerate((("g1bc", 2 * D), ("g2bc", 5 * D))):
        for half in range(2):
            bank = 3 + (gi * 2 + half) % 2
            P.op("pe", lambda e, bank=bank, c0=c0, half=half: e.matmul(
                PS[bank][:, :], lhsT=sel2[:, :], rhs=modrow[:, c0 + half * 512:c0 + (half + 1) * 512], start=True, stop=True),
                reads=["sel2"] + [("modrow", cb) for cb in range(12)], writes=[B(bank)])
            P.op("act", lambda e, bank=bank, gname=gname, half=half: e.activation(
                out=T[gname][:, half * 512:(half + 1) * 512], in_=PS[bank][:, :], func=AF.Identity),
                reads=[B(bank)], writes=[(gname, half)])
    modv = modT[:].rearrange("p (c t) -> p c t", t=2)
    P.op("dve", lambda e: e.tensor_scalar(out=mods[:, 0:8], in0=modv[:, 8:16, 0], scalar1=1.0, scalar2=None, op0=ALU.add),
         reads=["modT"], writes=[("mods", 0)])
    P.op("dve", lambda e: e.tensor_scalar(out=mods[:, 8:16], in0=modv[:, 8:16, 1], scalar1=1.0, scalar2=None, op0=ALU.add),
         reads=["modT"], writes=[("mods", 1)])
    P.op("dve", lambda e: e.tensor_scalar(out=mods[:, 16:24], in0=modv[:, 32:40, 0], scalar1=1.0, scalar2=1.0 / ALPHA,
                                          op0=ALU.add, op1=ALU.mult),
         reads=["modT"], writes=[("mods", 2)])
    P.barrier()

    WIN, WKR, WPOOL = T["WIN"], T["WKR"], T["WPOOL"]
    kvnT, KT, qnT, pooledT = T["kvnT"], T["KT"], T["qnT"], T["pooledT"]
    dma("pool", WIN[:], w_in.rearrange("(j p) n -> p j n", p=128), writes=["WIN"])
    dma("pool", WPOOL[:], w_pool.rearrange("g c d -> c g d"), writes=["WPOOL"])
    dma("sp", T["qg"][:], qg_fm, writes=["qg"])
    dma("sp", T["kvg"][:], kvg_fm, writes=["kvg"])
    dma("sp", T["psc"][:], pscale_fm, writes=["psc"])
    dma("sp", T["hm"][:], hmask, writes=["hm"])
    w_in_v = w_in.rearrange("(j p) n -> p j n", p=128)
    srcblk = [8, 0, 24, 16]
    for q in range(4):
        dma("sp", T["WKR32"][:, :, q * 8:(q + 1) * 8], w_in_v[:, :, 768 + srcblk[q]:768 + srcblk[q] + 8],
            writes=[("WKR32", q)], nonc=True)
    wkr32v = T["WKR32"][:].rearrange("p j (q k) -> p j q k", k=8)
    wkrv = WKR[:].rearrange("p j (q k) -> p j q k", k=8)
    for q in range(4):
        sgn = -1.0 if q % 2 == 0 else 1.0
        P.op("dve", lambda e, q=q, sgn=sgn: e.tensor_scalar(
            out=WKR[:, :, q * 8:(q + 1) * 8], in0=T["WKR32"][:, :, q * 8:(q + 1) * 8], scalar1=sgn, scalar2=None, op0=ALU.mult),
            reads=[("WKR32", q)], writes=[("WKR", q)])
    stg = T["stg32"]
    stg_kv = stg[:, 0:2, :]
    for half in range(2):
        dma("sp", stg[:, 0:2, 0:512], w_ukv.rearrange("(m p) n -> p m n", p=128)[:, :, half * 512:(half + 1) * 512],
            writes=["stg"])
        for m in range(2):
            P.op("dve", lambda e, m=m, half=half: e.tensor_scalar(
                out=T["WUKV"][:, m, half * 512:(half + 1) * 512], in0=stg[:, m, 0:512],
                scalar1=T["kvg"][:, m:m + 1], scalar2=None, op0=ALU.mult),
                reads=["stg", "kvg"], writes=[("WUKV", m, half)])
    dma("sp", stg[:], w_uq.rearrange("(m p) n -> p m n", p=128), reads=[("WUKV", 1, 1)], writes=["stg"])
    for m in range(4):
        P.op("dve", lambda e, m=m: e.tensor_scalar(out=T["WUQ"][:, m, :], in0=stg[:, m, :], scalar1=T["qg"][:, m:m + 1],
                                                   scalar2=None, op0=ALU.mult),
             reads=["stg", "qg"], writes=[("WUQ", m)])
        sv = stg[:, m, :].rearrange("p (h c) -> p h c", c=96)
        dv = T["WUQR"][:, m, :].rearrange("p (h c) -> p h c", c=32)
        for q in range(4):
            sgn = -1.0 if q % 2 == 0 else 1.0
            P.op("dve", lambda e, m=m, q=q, sgn=sgn, sv=sv, dv=dv: e.tensor_scalar(
                out=dv[:, :, q * 8:(q + 1) * 8], in0=sv[:, :, 64 + srcblk[q]:64 + srcblk[q] + 8],
                scalar1=T["qg"][:, m:m + 1], scalar2=sgn, op0=ALU.mult, op1=ALU.mult),
                reads=["stg", "qg"], writes=[("WUQR", m, q)])
    P.barrier()
    wuq_keys = [("WUQ", m) for m in range(4)]
    wuqr_keys = [("WUQR", m, q) for m in range(4) for q in range(4)]
    wukv_keys = [("WUKV", m, h) for m in range(2) for h in range(2)]

    xk_t = xk.rearrange("(t p) d -> t p d", p=128)
    xt_i = [0]

    def load_cs(blk, nt, buf):
        cs = T["cs%d" % buf]
        k = "cs%d" % buf
        dma("sp", cs[64:96, 0, 0:nt], cosk[:, blk * 512:blk * 512 + nt], writes=[k])
        dma("sp", cs[64:96, 1, 0:nt], sink[:, blk * 512:blk * 512 + nt], writes=[k])
        return cs, k

    def make_hT(blk, ntile, hT, hk, col):
        xts = []
        for t in range(ntile):
            i = xt_i[0] % 4
            xt_i[0] += 1
            dma("sp", T["xt%d" % i][:], xk_t[blk * 4 + t], writes=["xt%d" % i])
            xts.append(i)
        for j in range(8):
            bank = j % 2

            def fn(e, j=j, bank=bank):
                for t, i in enumerate(xts):
                    ins = e.transpose(out=PS[bank][:, t * 128:(t + 1) * 128], in_=T["xt%d" % i][:, j * 128:(j + 1) * 128],
                                      identity=idn[:])
                return ins
            P.op("pe", fn, reads=["ident"] + ["xt%d" % i for i in xts], writes=[B(bank)])
            P.op("act", lambda e, j=j, bank=bank: e.activation(
                out=hT[:, j, 0:ntile * 128], in_=PS[bank][:, 0:ntile * 128], func=AF.Identity,
                bias=modv[:, j, col:col + 1], scale=mods[:, col * 8 + j:col * 8 + j + 1]),
                reads=[B(bank), "modT", ("mods", col)], writes=[(hk, j)])

    def proj(hT, hk, nt, c0, M, bank, prow=0):
        def fn(e):
            for j in range(8):
                ins = e.matmul(PS[bank][prow:prow + M, 0:nt], lhsT=WIN[:, j, c0:c0 + M], rhs=hT[:, j, 0:nt],
                               start=(j == 0), stop=(j == 7))
            return ins
        P.op("pe", fn, reads=["WIN"] + [(hk, j) for j in range(8)], writes=[B(bank)])

    def rms_bcast(banks, nt, inv_n, sqk):
        sq = T["sq"]
        for i, bk in enumerate(banks):
            P.op("act", lambda e, i=i, bk=bk: e.activation(out=sq[:, i, 0:nt], in_=PS[bk][:, 0:nt], func=AF.Square),
                 reads=[B(bk)], writes=[("sq", i)])

        def fn(e):
            for i in range(len(banks)):
                ins = e.matmul(PS[6][:, 0:nt], lhsT=onesb[:], rhs=sq[:, i, 0:nt], start=(i == 0), stop=(i == len(banks) - 1))
            return ins
        P.op("pe", fn, reads=["onesb"] + [("sq", i) for i in range(len(banks))], writes=[B(6)])
        P.op("dve", lambda e: e.tensor_scalar(out=T["rbc"][:, 0:nt], in0=PS[6][:, 0:nt], scalar1=inv_n, scalar2=RMS_EPS,
                                              op0=ALU.mult, op1=ALU.add),
             reads=[B(6)], writes=["rbc"])
        P.op("act", lambda e: e.activation(out=T["rbc"][:, 0:nt], in_=T["rbc"][:, 0:nt], func=AF.Sqrt),
             reads=["rbc"], writes=["rbc"])
        P.op("dve", lambda e: e.reciprocal(out=T["rbc"][:, 0:nt], in_=T["rbc"][:, 0:nt]),
             reads=["rbc"], writes=["rbc"])

    def rope_combine(bankA, bankB, cs, csk, nt, dst_ap, dkey, eng="dve"):
        r1, r2 = T["rt1"], T["rt2"]
        P.op("dve", lambda e: e.tensor_tensor(out=r1[64:96, 0:nt], in0=PS[bankA][64:96, 0:nt], in1=cs[64:96, 0, 0:nt], op=ALU.mult),
             reads=[B(bankA), csk], writes=["rt1"])
        P.op("dve", lambda e: e.tensor_tensor(out=r2[64:96, 0:nt], in0=PS[bankB][64:96, 0:nt], in1=cs[64:96, 1, 0:nt], op=ALU.mult),
             reads=[B(bankB), csk], writes=["rt2"])
        P.op("pool", lambda e: e.tensor_tensor(out=dst_ap, in0=r1[64:96, 0:nt], in1=r2[64:96, 0:nt], op=ALU.add),
             reads=["rt1", "rt2"], writes=[dkey])

    pre = {}
    deferred = []

    order = [0, 4, 5, 6, 1, 7, 8, 9, 2, 10, 11, 12, 3, 13, 14, 15, 16]

    def stageA(pos):
        blk = order[pos]
        nt = 512 if blk < 16 else 256
        cs, csk = load_cs(blk, nt, pos % 2)
        make_hT(blk, nt // 128, T["hT%d" % (pos % 2)], "hT%d" % (pos % 2), 0 if blk < 16 else 1)
        pre[pos] = (cs, csk)
    stageA(0)
    for pos in range(17):
        blk = order[pos]
        own = blk < 4
        nt = 512 if blk < 16 else 256
        ntile = nt // 128
        col = 0 if blk < 16 else 1
        hT = T["hT%d" % (pos % 2)]
        hk = "hT%d" % (pos % 2)
        cs, csk = pre[pos]
        ksl = slice(blk * 512, blk * 512 + nt)
        proj(hT, hk, nt, 512, 128, 2)
        proj(hT, hk, nt, 640, 128, 3)
        proj(hT, hk, nt, 768, 32, 4, prow=64)

        def fnB(e, hT=hT, nt=nt):
            for j in range(8):
                ins = e.matmul(PS[5][64:96, 0:nt], lhsT=WKR[:, j, :], rhs=hT[:, j, 0:nt], start=(j == 0), stop=(j == 7))
            return ins
        P.op("pe", fnB, reads=[("WKR", q) for q in range(4)] + [(hk, j) for j in range(8)], writes=[B(5)])
        for m in range(2):
            P.op("act", lambda e, m=m, nt=nt: e.activation(out=T["latsb"][:, m, 0:nt], in_=PS[2 + m][:, 0:nt], func=AF.Identity),
                 reads=[B(2 + m)], writes=[("latsb", m)])
        rms_bcast([2, 3], nt, 1.0 / 256, "sq")
        if pos + 1 < 17:
            stageA(pos + 1)
        for m in range(2):
            P.op("dve", lambda e, m=m, ksl=ksl, nt=nt: e.tensor_tensor(out=kvnT[:, m, ksl], in0=T["latsb"][:, m, 0:nt],
                                                                      in1=T["rbc"][:, 0:nt], op=ALU.mult),
                 reads=[("latsb", m), "rbc"], writes=[("kvnT", m, blk)])
        rope_combine(4, 5, cs, csk, nt, KT[64:96, ksl], ("KTr", blk))
        deferred_new = []
        if not own and pos % 4 == 3:
            for f_ in deferred:
                f_()
            deferred.clear()
        if own:
            qsl = slice(blk * 512, (blk + 1) * 512)
            for m in range(4):
                proj(hT, hk, 512, m * 128, 128, 2 + m)
            rms_bcast([2, 3, 4, 5], 512, 1.0 / 512, "sq")
            for m in range(4):
                P.op("dve", lambda e, m=m, qsl=qsl: e.tensor_tensor(out=qnT[:, m, qsl], in0=PS[2 + m][:, :],
                                                                    in1=T["rbc"][:, :], op=ALU.mult),
                     reads=[B(2 + m), "rbc"], writes=[("qnT", m, blk)])
            uext, pa, pb = T["uext"], T["pa"], T["pb"]
            dma("sp", T["xh"][:], xhalo[blk], writes=["xh"])
            for j in range(8):
                bank = j % 2
                P.op("pe", lambda e, j=j, bank=bank: e.transpose(out=PS[bank][:, 0:16], in_=T["xh"][:, j * 128:(j + 1) * 128],
                                                                identity=idn[0:16, 0:16]),
                     reads=["ident", "xh"], writes=[B(bank)])
                P.op("act", lambda e, j=j, bank=bank: e.activation(
                    out=T["hTh"][:, j, :], in_=PS[bank][:, 0:16], func=AF.Identity,
                    bias=modv[:, j, 0:1], scale=mods[:, j:j + 1]),
                    reads=[B(bank), "modT", ("mods", 0)], writes=[("hTh", j)])
            for g in range(4):
                bank = 2 + g
                proj(hT, hk, 512, 800 + g * 128, 128, bank)
                P.op("act", lambda e, g=g, bank=bank: e.activation(out=uext[:, g, 8:520], in_=PS[bank][:, :], func=AF.Identity),
                     reads=[B(bank)], writes=[("uext", g, 1)])
            for g in range(4):
                def fn(e, g=g):
                    for j in range(8):
                        ins = e.matmul(PS[7][:, g * 16:(g + 1) * 16], lhsT=WIN[:, j, 800 + g * 128:800 + (g + 1) * 128],
                                       rhs=T["hTh"][:, j, :], start=(j == 0), stop=(j == 7))
                    return ins
                P.op("pe", fn, reads=["WIN"] + [("hTh", j) for j in range(8)], writes=[B(7)])
            p7 = PS[7][:, 0:64].rearrange("p (g k) -> p g k", k=16)
            P.op("dve", lambda e, blk=blk: e.tensor_tensor(out=uext[:, :, 0:8], in0=p7[:, :, 0:8],
                                                           in1=bc_mid(T["hm"][:, blk, 0:8], 4), op=ALU.mult),
                 reads=[B(7), "hm"], writes=[("uext", 0, 0), ("uext", 1, 0), ("uext", 2, 0), ("uext", 3, 0)])
            P.op("dve", lambda e, blk=blk: e.tensor_tensor(out=uext[:, :, 520:528], in0=p7[:, :, 8:16],
                                                           in1=bc_mid(T["hm"][:, blk, 8:16], 4), op=ALU.mult),
                 reads=[B(7), "hm"], writes=[("uext", 0, 2), ("uext", 1, 2), ("uext", 2, 2), ("uext", 3, 2)])
            for f_ in deferred:
                f_()
            deferred.clear()
            def chain(src_ap_fn, src_keys, g, tag):
                cur, ckeys, ln, step = src_ap_fn, src_keys, 528, 1
                bufs = [pa, pb]
                bkeys = ["pa", "pb"]
                k = 0
                for lvl in range(g + 1):
                    dst = bufs[k % 2]
                    dk = bkeys[k % 2]
                    ln2 = ln - step
                    P.op("pool", lambda e, cur=cur, dst=dst, ln2=ln2, step=step: e.tensor_tensor(
                        out=dst[:, 0:ln2], in0=cur(0, ln2), in1=cur(step, step + ln2), op=ALU.add),
                        reads=list(ckeys), writes=[dk])
                    cur = (lambda dst: (lambda a, b: dst[:, a:b]))(dst)
                    ckeys = [dk]
                    ln, step = ln2, step * 2
                    k += 1
                return cur, ckeys
            for g in range(4):
                half = 1 << g
                o = 8 - half
                ic = T["ic%d" % (g % 2)]
                ick = "ic%d" % (g % 2)
                dma("sp", ic[:], icntc[blk * 4 + g:blk * 4 + g + 1, :].partition_broadcast(128), writes=[ick])
                cur, ck = chain(lambda a, b, g=g: uext[:, g, a:b], [("uext", g, i) for i in range(3)], g, "u")
                P.op("dve", lambda e, g=g, cur=cur, o=o, ic=ic: e.tensor_tensor(out=ic[:], in0=cur(o, o + 512), in1=ic[:], op=ALU.mult),
                     reads=list(ck) + [ick], writes=[ick])
                P.op("dve", lambda e, g=g, ic=ic: e.tensor_tensor(out=T["pin"][:, g, :], in0=ic[:], in1=uext[:, g, 8:520],
                                                                  op=ALU.subtract),
                     reads=[ick, ("uext", g, 1)], writes=[("pin", g)])
                bank = 2 + g

                def later(g=g, bank=bank, qsl=qsl, blk=blk):
                    P.op("pe", lambda e: e.matmul(PS[bank][:, :], lhsT=WPOOL[:, g, :], rhs=T["pin"][:, g, :], start=True, stop=True),
                         reads=["WPOOL", ("pin", g)], writes=[B(bank)])
                    P.op("act", lambda e: e.activation(out=pooledT[:, g, qsl], in_=PS[bank][:, :],
                                                       func=AF.Identity, scale=T["psc"][:, g:g + 1]),
                         reads=[B(bank), "psc"], writes=[("pooledT", g, blk)])
                deferred_new.append(later)
            deferred.extend(deferred_new)
    for f_ in deferred:
        f_()
    deferred.clear()
    dump("kvnT", kvnT[:, 0, :], reads=[("kvnT", 0, b_) for b_ in range(17)])
    P.barrier()

    attnT, QH = T["attnT"], T["QH"]
    WUKV, WUQ, WUQR = T["WUKV"], T["WUQ"], T["WUQR"]
    P.op("pool", lambda e: e.memset(T["c1row"][:], 1.0), writes=["c1row"])
    for i in range(2):
        P.op("pool", lambda e, i=i: e.memset(T["VH%d" % i][:, :, 64:65], 1.0), writes=[("VH1s", i)])
    pt_i = [0]
    s_i = [0]
    o_i = [0]
    for h in range(NH):
        VH = T["VH%d" % (h % 2)]
        vk = ("VH", h % 2)
        for blk in range(17):
            nt = 512 if blk < 16 else 256
            ksl = slice(blk * 512, blk * 512 + nt)
            bank = 4 + blk % 2

            def fn(e, ksl=ksl, nt=nt, bank=bank, h=h):
                for m in range(2):
                    ins = e.matmul(PS[bank][0:64, 0:nt], lhsT=WUKV[:, m, h * 128:h * 128 + 64], rhs=kvnT[:, m, ksl],
                                   start=(m == 0), stop=(m == 1))
                return ins
            P.op("pe", fn, reads=[], writes=[B(bank)])
            eng = "dve"
            if eng == "act":
                P.op("act", lambda e, ksl=ksl, nt=nt, bank=bank: e.activation(out=KT[0:64, ksl], in_=PS[bank][0:64, 0:nt], func=AF.Identity),
                     reads=[B(bank)], writes=[("KTn", blk)])
            else:
                P.op("dve", lambda e, ksl=ksl, nt=nt, bank=bank: e.tensor_copy(out=KT[0:64, ksl], in_=PS[bank][0:64, 0:nt]),
                     reads=[B(bank)], writes=[("KTn", blk)])
        for c0 in range(0, 66, 8):
            ncz = min(8, 66 - c0)
            bank = 4 + (c0 // 8) % 2

            def fn(e, c0=c0, ncz=ncz, bank=bank, h=h):
                for cc in range(ncz):
                    c = c0 + cc
                    for m in range(2):
                        ins = e.matmul(PS[bank][:, cc * 64:(cc + 1) * 64], lhsT=kvnT[:, m, c * 128:(c + 1) * 128],
                                       rhs=WUKV[:, m, h * 128 + 64:h * 128 + 128], start=(m == 0), stop=(m == 1))
                return ins
            P.op("pe", fn, reads=[], writes=[B(bank)])
            P.op("dve", lambda e, c0=c0, ncz=ncz, bank=bank, VH=VH: e.tensor_copy(
                out=VH[:, c0:c0 + ncz, 0:64], in_=PS[bank][:, 0:ncz * 64].rearrange("p (c v) -> p c v", v=64)),
                reads=[B(bank)], writes=[(vk, c0 // 8)])
        for qb in range(4):
            qsl = slice(qb * 512, (qb + 1) * 512)
            cs, csk = load_cs(qb, 512, qb % 2)

            def fnA(e, qsl=qsl, h=h):
                for m in range(4):
                    ins = e.matmul(PS[4][0:96, :], lhsT=WUQ[:, m, h * 96:(h + 1) * 96], rhs=qnT[:, m, qsl],
                                   start=(m == 0), stop=(m == 3))
                return ins
            P.op("pe", fnA, reads=[], writes=[B(4)])

            def fnB(e, qsl=qsl, h=h):
                for m in range(4):
                    ins = e.matmul(PS[5][64:96, :], lhsT=WUQR[:, m, h * 32:(h + 1) * 32], rhs=qnT[:, m, qsl],
                                   start=(m == 0), stop=(m == 3))
                return ins
            P.op("pe", fnB, reads=[], writes=[B(5)])
            P.op("dve", lambda e, qsl=qsl: e.tensor_copy(out=QH[0:64, qsl], in_=PS[4][0:64, :]),
                 reads=[B(4)], writes=[("QHn", qb)])
            rope_combine(4, 5, cs, csk, 512, QH[64:96, qsl], ("QHr", qb))
        for qb in range(4):
            qsl = slice(qb * 512, (qb + 1) * 512)
            ob = 6 + (o_i[0] % 2)
            o_i[0] += 1

            def s_mms(e, p, sp, qsl=qsl):
                for i in range(2):
                    c = 2 * p + i
                    ins = e.matmul(PSP[sp][:, i * 512:(i + 1) * 512], lhsT=KT[0:96, c * 128:(c + 1) * 128], rhs=QH[0:96, qsl],
                                   start=True, stop=True)
                return ins

            def pv_mms(e, p, pt, ob=ob, VH=VH):
                for i in range(2):
                    c = 2 * p + i
                    ins = e.matmul(PS[ob][0:65, :], lhsT=VH[:, c, 0:65], rhs=T["PTP%d" % pt][:, i * 512:(i + 1) * 512],
                                   start=(c == 0), stop=(c == 65))
                return ins

            def s_keys(p):
                return [("KTn", p // 2), ("KTr", p // 2), ("QHn", qb), ("QHr", qb)]

            def rec_exp(sp, pt):
                P.op("act", lambda e, sp=sp, pt=pt: e.activation(out=T["PTP%d" % pt][:], in_=PSP[sp][:, :], func=AF.Exp, scale=ATTN_SCALE),
                     reads=[B(2 * sp), B(2 * sp + 1)], writes=[("PTP", pt)])
            slot = {}
            for p in range(3):
                sp, pt = s_i[0] % 3, pt_i[0] % 3
                s_i[0] += 1
                pt_i[0] += 1
                slot[p] = (sp, pt)
                P.op("pe", lambda e, p=p, sp=sp, s_mms=s_mms: s_mms(e, p, sp), reads=s_keys(p), writes=[B(2 * sp), B(2 * sp + 1)])
                rec_exp(sp, pt)
            for p in range(33):
                sp0, pt0 = slot.pop(p)
                if p + 3 < 33:
                    sp, pt = s_i[0] % 3, pt_i[0] % 3
                    s_i[0] += 1
                    pt_i[0] += 1
                    slot[p + 3] = (sp, pt)

                    def fn(e, p=p, pt0=pt0, sp=sp, pv_mms=pv_mms, s_mms=s_mms):
                        pv_mms(e, p, pt0)
                        return s_mms(e, p + 3, sp)
                    P.op("pe", fn, reads=[("PTP", pt0), (vk, (2 * p) // 8), ("VH1s", h % 2)] + s_keys(p + 3),
                         writes=[B(ob), B(2 * sp), B(2 * sp + 1)])
                    rec_exp(sp, pt)
                else:
                    P.op("pe", lambda e, p=p, pt0=pt0, pv_mms=pv_mms: pv_mms(e, p, pt0),
                         reads=[("PTP", pt0), (vk, (2 * p) // 8), ("VH1s", h % 2)], writes=[B(ob)])
            rden, num = T["rden"], T["num"]
            P.op("dve", lambda e, ob=ob: e.reciprocal(out=rden[64:65, :], in_=PS[ob][64:65, :]), reads=[B(ob)], writes=["rden"])
            P.op("dve", lambda e, ob=ob: e.tensor_copy(out=num[:, :], in_=PS[ob][0:64, :]), reads=[B(ob)], writes=["num"])
            P.op("pe", lambda e: e.matmul(PS[5][0:64, :], lhsT=T["c1row"][64:65, 0:64], rhs=rden[64:65, :], start=True, stop=True),
                 reads=["rden", "c1row"], writes=[B(5)])
            P.op("dve", lambda e, h=h, qsl=qsl: e.tensor_tensor(out=attnT[:, h, qsl], in0=num[:, :], in1=PS[5][0:64, :], op=ALU.mult),
                 reads=["num", B(5)], writes=[("attnT", h, qb)])
    dump("attnT", attnT[:, 0, :], reads=[("attnT", 0, q) for q in range(4)])
    P.barrier()

    WOA, WOP, acc = T["WOA"], T["WOP"], T["acc"]
    dma("pool", WOA[:], w_out[0:512, :].rearrange("(h p) n -> p h n", p=64), writes=["WOA"])
    dma("pool", WOP[:], w_out[512:1024, :].rearrange("(g p) n -> p g n", p=128), writes=["WOP"])
    dma("sp", T["l1g"][:], ln1_g.partition_broadcast(128), writes=["l1g"])
    dma("sp", T["l1b"][:], ln1_b.partition_broadcast(128), writes=["l1b"])
    P.op("pool", lambda e: e.tensor_scalar(out=T["l1g"][:], in0=T["l1g"][:], scalar1=ALPHA, scalar2=None, op0=ALU.mult), reads=["l1g"], writes=["l1g"])
    P.op("pool", lambda e: e.tensor_scalar(out=T["l1b"][:], in0=T["l1b"][:], scalar1=ALPHA, scalar2=None, op0=ALU.mult), reads=["l1b"], writes=["l1b"])
    P.op("dve", lambda e: e.memset(T["stat"][:], 0.0), writes=["statz"])
    def p3_mm(t):
        tsl = slice(t * 128, (t + 1) * 128)
        for half in range(2):
            bank = (t % 2) * 2 + half

            def fn(e, tsl=tsl, half=half, bank=bank):
                for h in range(8):
                    e.matmul(PS[bank][:, :], lhsT=attnT[:, h, tsl], rhs=WOA[:, h, half * 512:(half + 1) * 512],
                             start=(h == 0), stop=False)
                for g in range(4):
                    ins = e.matmul(PS[bank][:, :], lhsT=pooledT[:, g, tsl], rhs=WOP[:, g, half * 512:(half + 1) * 512],
                                   start=False, stop=(g == 3))
                return ins
            P.op("pe", fn, reads=["WOA", "WOP"], writes=[B(bank)])
            hs = slice(half * 512, (half + 1) * 512)
            P.op("dve", lambda e, bank=bank, hs=hs, t=t: e.tensor_tensor(out=acc[:, t, hs], in0=PS[bank][:, :], in1=T["g1bc"][:, hs], op=ALU.mult),
                 reads=[B(bank)], writes=[("y", t, half)])

    def p3_ln(t):
        xs = T["xs%d" % (t % 4)]
        xsk = "xs%d" % (t % 4)
        dma("sp", xs[:], xk_t[t], writes=[xsk])
        P.op("dve", lambda e, xs=xs, t=t: e.scalar_tensor_tensor(out=xs[:], in0=xs[:], scalar=ALPHA, in1=acc[:, t, :], op0=ALU.mult, op1=ALU.add),
             reads=[xsk, ("y", t, 0), ("y", t, 1)], writes=[xsk])
        ln_tile(xs[:], acc[:, t, :], T["l1g"][:], T["l1b"][:], "st3", t, xsk, ("acc", t), extra_reads=["l1g", "l1b"])
    for t in range(17):
        if t < 16:
            p3_mm(t)
        if t >= 1:
            p3_ln(t - 1)
    dump("x1", acc[:, 0, :], reads=[("acc", 0)])
    P.barrier()

    h2T, wr = T["h2T"], T["wr"]
    dma("sp", wr[:], w_router.rearrange("(j p) n -> p j n", p=128), writes=["wr"])
    dma("sp", T["rb"][:], rbias.partition_broadcast(128), writes=["rb"])
    scores, gates = T["scores"], T["gates"]
    g2bc = T["g2bc"]
    weg = w_eg.rearrange("e (j p) f -> e p j f", p=128)
    weu = w_eu.rearrange("e (j p) f -> e p j f", p=128)
    wed = w_ed.rearrange("e (c p) d -> e p c d", p=128)

    def load_w(e_):
        i = e_ % 2
        dma("pool", T["wg%d" % i][:], weg[e_], writes=[("wg", i)])
        dma("pool", T["wu%d" % i][:], weu[e_], writes=[("wu", i)])
        dma("sp", T["wd32_%d" % i][:], wed[e_], writes=[("wd32", i)])
        P.op("pool", lambda e, i=i: e.tensor_tensor(out=T["wd%d" % i][:], in0=T["wd32_%d" % i][:], in1=bc_mid(g2bc[:], 2), op=ALU.mult),
             reads=[("wd32", i)], writes=[("wd", i)])
    load_w(0)
    def p4_front(t):
        tsl = slice(t * 128, (t + 1) * 128)
        h2f = T["h2f%d" % (t % 2)]
        pp = t % 2

        def fnT(e, t=t, pp=pp):
            for j in range(8):
                ins = e.transpose(out=PSP[pp][:, j * 128:(j + 1) * 128], in_=acc[:, t, j * 128:(j + 1) * 128], identity=idn[:])
            return ins
        P.op("pe", fnT, reads=["ident", ("acc", t)], writes=[B(2 * pp), B(2 * pp + 1)])
        for j in range(8):
            P.op("act", lambda e, j=j, pp=pp, h2f=h2f: e.activation(out=h2f[:, j, :], in_=PSP[pp][:, j * 128:(j + 1) * 128], func=AF.Identity,
                                                                    bias=modv[:, 24 + j, 0:1], scale=mods[:, 16 + j:16 + j + 1]),
                 reads=[B(2 * pp), B(2 * pp + 1)], writes=[("h2f", t % 2, j)])
        P.op("dve", lambda e, tsl=tsl, h2f=h2f: e.tensor_copy(out=h2T[:, :, tsl], in_=h2f[:, :, :]),
             reads=[("h2f", t % 2, j) for j in range(8)], writes=[("h2T", t)])

    def p4_back(t):
        h2f = T["h2f%d" % (t % 2)]

        def fn(e, t=t, h2f=h2f):
            for j in range(8):
                ins = e.matmul(PS[4 + t % 2][:, 0:NEXP], lhsT=h2f[:, j, :], rhs=wr[:, j, :], start=(j == 0), stop=(j == 7))
            return ins
        P.op("pe", fn, reads=["wr"] + [("h2f", t % 2, j) for j in range(8)], writes=[B(4 + t % 2)])
        P.op("act", lambda e, t=t: e.activation(out=scores[:, t, :], in_=PS[4 + t % 2][:, 0:NEXP], func=AF.Sigmoid),
             reads=[B(4 + t % 2)], writes=[("scores", t)])
    for t in range(17):
        if t < 16:
            p4_front(t)
        if t >= 1:
            p4_back(t - 1)
    sk = [("scores", t) for t in range(16)]
    ra, rbb, rc = T["ra"], T["rbb"], T["rc"]
    rs, rs2, rs3, rs4, t8 = T["rs"], T["rs2"], T["rs3"], T["rs4"], T["t8"]
    v4 = lambda a: a[:].rearrange("p t (g k) -> p (t g) k", k=8)
    f2 = lambda a: a[:].rearrange("p t g -> p (t g)")
    P.op("dve", lambda e: e.tensor_tensor(out=ra[:], in0=scores[:], in1=bc_mid(T["rb"][:], 16), op=ALU.add),
         reads=sk + ["rb"], writes=["ra"])
    P.op("dve", lambda e: e.tensor_reduce(out=f2(rs), in_=v4(ra), axis=AX.X, op=ALU.max), reads=["ra"], writes=["rs"])
    P.op("dve", lambda e: e.tensor_tensor(out=v4(rbb), in0=v4(ra), in1=bc_last(f2(rs), 8), op=ALU.is_equal),
         reads=["ra", "rs"], writes=["rbb"])
    P.op("dve", lambda e: e.scalar_tensor_tensor(out=rbb[:], in0=rbb[:], scalar=-10.0, in1=ra[:], op0=ALU.mult, op1=ALU.add),
         reads=["rbb", "ra"], writes=["rbb"])
    P.op("dve", lambda e: e.tensor_reduce(out=f2(rs2), in_=v4(rbb), axis=AX.X, op=ALU.max), reads=["rbb"], writes=["rs2"])
    P.op("dve", lambda e: e.tensor_tensor(out=rs[:], in0=rs[:], in1=rs2[:], op=ALU.add), reads=["rs", "rs2"], writes=["rs"])
    for t in range(16):
        P.op("dve", lambda e, t=t: e.max(out=t8[:, t, :], in_=rs[:, t, :]), reads=["rs"], writes=[("t8", t)])
    t8k = [("t8", t) for t in range(16)]
    P.op("dve", lambda e: e.tensor_tensor(out=rs3[:], in0=rs[:], in1=bc_last(t8[:, :, 3], 8), op=ALU.is_ge),
         reads=["rs"] + t8k, writes=["rs3"])
    P.op("dve", lambda e: e.tensor_scalar(out=rs4[:], in0=rs3[:], scalar1=10.0, scalar2=-10.0, op0=ALU.mult, op1=ALU.add),
         reads=["rs3"], writes=["rs4"])
    P.op("dve", lambda e: e.tensor_tensor(out=v4(rbb), in0=v4(ra), in1=bc_last(f2(rs3), 8), op=ALU.mult),
         reads=["ra", "rs3", "rs2"], writes=["rbb"])
    P.op("dve", lambda e: e.tensor_tensor(out=v4(rbb), in0=v4(rbb), in1=bc_last(f2(rs4), 8), op=ALU.add),
         reads=["rbb", "rs4"], writes=["rbb"])
    for t in range(16):
        P.op("dve", lambda e, t=t: e.max(out=t8[:, t, :], in_=rbb[:, t, :]), reads=["rbb", "rs3"], writes=[("t8", t)])
    P.op("dve", lambda e: e.tensor_tensor(out=rc[:], in0=rbb[:], in1=bc_last(t8[:, :, 7], NEXP), op=ALU.is_ge),
         reads=["rbb"] + t8k, writes=["rc"])
    P.op("dve", lambda e: e.tensor_tensor(out=rc[:], in0=rc[:], in1=scores[:], op=ALU.mult), reads=["rc"] + sk, writes=["rc"])
    P.op("dve", lambda e: e.tensor_reduce(out=T["rsum"][:], in_=rc[:], axis=AX.X, op=ALU.add), reads=["rc"], writes=["rsum"])
    P.op("dve", lambda e: e.reciprocal(out=T["rsum"][:], in_=T["rsum"][:]), reads=["rsum"], writes=["rsum"])
    P.op("dve", lambda e: e.scalar_tensor_tensor(out=gates[:], in0=rc[:], scalar=2.5, in1=bc_last(T["rsum"][:], NEXP),
                                                 op0=ALU.mult, op1=ALU.mult),
         reads=["rc", "rsum"], writes=["gates"])
    dump("gates", gates[:, 0, :], reads=["gates"])
    P.barrier()

    y_i = [0]
    u_i = [0]

    def gu_half(e_, blk, u, c):
        i = e_ % 2
        tsl = slice(blk * 512, (blk + 1) * 512)
        aT = T["aT%d" % u]
        bg, bu = c * 2, c * 2 + 1

        def fn(e):
            for j in range(8):
                e.matmul(PS[bg][:, :], lhsT=T["wg%d" % i][:, j, c * 128:(c + 1) * 128], rhs=h2T[:, j, tsl], start=(j == 0), stop=(j == 7))
            for j in range(8):
                ins = e.matmul(PS[bu][:, :], lhsT=T["wu%d" % i][:, j, c * 128:(c + 1) * 128], rhs=h2T[:, j, tsl], start=(j == 0), stop=(j == 7))
            return ins
        P.op("pe", fn, reads=[("wg", i), ("wu", i)], writes=[B(bg), B(bu)])
        sg = T["sg%d" % c]
        P.op("act", lambda e: e.activation(out=sg[:], in_=PS[bg][:, :], func=AF.Silu), reads=[B(bg)], writes=[("sg", c)])
        P.op("dve", lambda e: e.tensor_tensor(out=aT[:, c, :], in0=sg[:], in1=PS[bu][:, :], op=ALU.mult),
             reads=[("sg", c), B(bu)], writes=[("aT", u, c)])

    def down_half(e_, blk, u, hh):
        i = e_ % 2
        aT = T["aT%d" % u]

        def fn(e):
            for k in range(2):
                tt = hh * 2 + k
                for half in range(2):
                    bank = 4 + k * 2 + half
                    for c in range(2):
                        ins = e.matmul(PS[bank][:, :], lhsT=aT[:, c, tt * 128:(tt + 1) * 128],
                                       rhs=T["wd%d" % i][:, c, half * 512:(half + 1) * 512], start=(c == 0), stop=(c == 1))
            return ins
        P.op("pe", fn, reads=[("aT", u, 0), ("aT", u, 1), ("wd", i)], writes=[B(4), B(5), B(6), B(7)])
        for k in range(2):
            t = blk * 4 + hh * 2 + k
            for half in range(2):
                bank = 4 + k * 2 + half
                hs = slice(half * 512, (half + 1) * 512)
                if e_ < NEXP:
                    P.op("dve", lambda e, t=t, hs=hs, bank=bank: e.scalar_tensor_tensor(
                        out=acc[:, t, hs], in0=PS[bank][:, :], scalar=gates[:, t, e_:e_ + 1], in1=acc[:, t, hs],
                        op0=ALU.mult, op1=ALU.add),
                        reads=[B(bank)], writes=[("acc", t, half)])
                else:
                    P.op("dve", lambda e, t=t, hs=hs, bank=bank: e.tensor_tensor(
                        out=acc[:, t, hs], in0=PS[bank][:, :], in1=acc[:, t, hs], op=ALU.add),
                        reads=[B(bank)], writes=[("acc", t, half)])

    dma("sp", T["l2g"][:], ln2_g.partition_broadcast(128), writes=["l2g"])
    dma("sp", T["l2b"][:], ln2_b.partition_broadcast(128), writes=["l2b"])
    prev = None
    u_i = [0]
    for e_ in range(NEXP + 1):
        for blk in range(4):
            u = u_i[0] % 2
            u_i[0] += 1
            gu_half(e_, blk, u, 0)
            if prev is not None:
                down_half(*prev, 0)
            gu_half(e_, blk, u, 1)
            if prev is not None:
                down_half(*prev, 1)
            prev = (e_, blk, u)
            if blk == 0 and e_ + 1 <= NEXP:
                load_w(e_ + 1)
    down_half(*prev, 0)
    down_half(*prev, 1)

    finals = []
    P.op("dve", lambda e: e.memset(T["stat"][:], 0.0), writes=["statz"])
    for t in range(16):
        o = T["o%d" % (t % 4)]
        ok = "o%d" % (t % 4)
        ln_tile(acc[:, t, :], o[:], T["l2g"][:], T["l2b"][:], "st6", t, ("acc", t, 0), ok, extra_reads=["l2g", "l2b", ("acc", t, 1)])
        finals.append(dma("sp", yout[t * 128:(t + 1) * 128, :], o[:], reads=[ok], writes=[("yout", t % 4)]))
    P.op("sp", None, extra_deps=finals + [i for i, o in enumerate(P.ops) if o["dma"] and isinstance(o["chan"], tuple) and o["chan"][0] == "dbg"])

    P.finalize(nc, st)
    with nc.Block() as block:
        @block.sync
        def _(e):
            P.emit("sp", e)

        @block.scalar
        def _(e):
            P.emit("act", e)

        @block.vector
        def _(e):
            P.emit("dve", e)

        @block.gpsimd
        def _(e):
            P.emit("pool", e)

        @block.tensor
        def _(e):
            P.emit("pe", e)
    st.close()
    return nc, P


def _rope_tables(own0):
    t_own = np.arange(own0, own0 + TOWN)
    t_rest = np.concatenate([np.arange(0, own0), np.arange(own0 + TOWN, SEQ)])
    t = np.concatenate([t_own, t_rest]).astype(np.int64)
    pos = np.stack([t // 64, t % 64], axis=0).astype(np.float32)
    inv = (np.float32(10000.0) ** (-np.arange(8, dtype=np.float32) / np.float32(8.0))).astype(np.float32)
    ang = pos[:, None, :] * inv[None, :, None]
    cos = np.cos(ang).astype(np.float32)
    sin = np.sin(ang).astype(np.float32)
    cos32 = np.stack([cos[a, f] for a in range(2) for hf in range(2) for f in range(8)], 0)
    sin32 = np.stack([sin[a, f] for a in range(2) for hf in range(2) for f in range(8)], 0)
    cosk = np.concatenate([cos32, np.ones((32, CTX), np.float32)], 1)
    sink = np.concatenate([sin32, np.zeros((32, CTX), np.float32)], 1)
    return np.ascontiguousarray(cosk), np.ascontiguousarray(sink)


def make_in_maps(x, c, ctx, c_ctx, w_ada, b_ada, w_in, q_norm_g, w_uq, kv_norm_g, w_ukv,
                 w_pool, pool_scale, w_out, ln1_g, ln1_b, w_router, router_bias,
                 w_e_gate, w_e_up, w_e_down, w_s_gate, w_s_up, w_s_down, ln2_g, ln2_b):
    f = lambda a: np.ascontiguousarray(np.asarray(a, dtype=np.float32))
    x, c, ctx, c_ctx = f(x), f(c), f(ctx), f(c_ctx)
    fm = lambda v, n: np.ascontiguousarray(f(v).reshape(n, 128).T)
    shared = dict(
        ident=np.eye(128, dtype=np.float32),
        w_ada=f(w_ada[0]), b_ada_fm=fm(b_ada[0], 48), b_ada_row=f(b_ada[0]).reshape(1, -1),
        w_in=f(w_in[0]), qg_fm=fm(q_norm_g[0], 4), kvg_fm=fm(kv_norm_g[0], 2),
        w_uq=f(w_uq[0]), w_ukv=f(w_ukv[0]), w_pool=f(w_pool[0]), pscale_fm=fm(pool_scale[0], 4),
        w_out=f(w_out[0]), ln1_g=f(ln1_g[0]).reshape(1, -1), ln1_b=f(ln1_b[0]).reshape(1, -1),
        ln2_g=f(ln2_g[0]).reshape(1, -1), ln2_b=f(ln2_b[0]).reshape(1, -1),
        w_router=f(w_router[0]), rbias=f(router_bias[0]).reshape(1, -1),
        w_eg=np.concatenate([f(w_e_gate[0]), f(w_s_gate[0])[None]], 0),
        w_eu=np.concatenate([f(w_e_up[0]), f(w_s_up[0])[None]], 0),
        w_ed=np.concatenate([f(w_e_down[0]), f(w_s_down[0])[None]], 0),
    )
    maps = []
    for core in range(NCORE):
        b, own0 = core // 4, (core % 4) * TOWN
        xb = x[b]
        xk = np.concatenate([xb[own0:own0 + TOWN], xb[:own0], xb[own0 + TOWN:], ctx[b]], 0)
        xh = np.zeros((4, 16, D), np.float32)
        hm = np.zeros((128, 4, 16), np.float32)
        for blk in range(4):
            s0 = own0 + blk * 512
            for k in range(8):
                tl, tr = s0 - 8 + k, s0 + 512 + k
                if 0 <= tl < SEQ:
                    xh[blk, k] = xb[tl]
                    hm[:, blk, k] = 1.0
                if 0 <= tr < SEQ:
                    xh[blk, 8 + k] = xb[tr]
                    hm[:, blk, 8 + k] = 1.0
        ic = np.zeros((16, 512), np.float32)
        for blk in range(4):
            tok = own0 + blk * 512 + np.arange(512)
            for g in range(4):
                w = 2 << g
                lo = np.clip(tok - w // 2, 0, SEQ)
                hi = np.clip(tok - w // 2 + w, 0, SEQ)
                ic[blk * 4 + g] = (np.float32(1.0) / (hi - lo).astype(np.float32))
        cosk, sink = _rope_tables(own0)
        cv = np.stack([fm(c[b], 8), fm(c_ctx, 8)], axis=-1)
        m = dict(shared)
        m.update(xk=np.ascontiguousarray(xk), xhalo=xh, hmask=hm, cosk=cosk, sink=sink, cvec=np.ascontiguousarray(cv), icntc=ic)
        maps.append(m)
    return maps


_CACHE = {}


def kernel(**inputs):
    if "nc" not in _CACHE:
        _CACHE["nc"] = build_program()[0]
    nc = _CACHE["nc"]
    in_maps = make_in_maps(**inputs)
    res = run_bass_kernel_spmd(nc, in_maps, core_ids=list(range(NCORE)))
    out = np.zeros((2, SEQ, D), np.float32)
    for core in range(NCORE):
        b, own0 = core // 4, (core % 4) * TOWN
        out[b, own0:own0 + TOWN] = res.results[core]["yout"]
    return out
```

```python
import math
from contextlib import ExitStack

import numpy as np
import concourse.bass as bass
import concourse.mybir as mybir
from concourse.bass_utils import run_bass_kernel_spmd

F32 = mybir.dt.float32
BF16 = mybir.dt.bfloat16
AF = mybir.ActivationFunctionType
ALU = mybir.AluOpType
AX = mybir.AxisListType

D = 1024
SEQ = 8192
NCORE = 8
TOWN = 2048
CTX = 256
NKEY = SEQ + CTX
NH = 8
ATTN_SCALE = 1.0 / math.sqrt(96.0)
ALPHA = 2.0 ** 0.25
LN_EPS = 1e-5
RMS_EPS = 1e-6
NEXP = 64
SBUF_BASE = 16512
SBUF_LIMIT = 229376


class Prog:
    ENGS = ("pe", "act", "dve", "pool", "sp")

    def __init__(self):
        self.ops = []
        self.res_w = {}
        self.res_r = {}
        self.last_eng = {}
        self.last_chan = {}

    def op(self, eng, fn, reads=(), writes=(), dma=False, extra_deps=()):
        idx = len(self.ops)
        deps = set(extra_deps)
        for r in reads:
            w = self.res_w.get(r)
            if w is not None:
                deps.add(w)
        for k in writes:
            w = self.res_w.get(k)
            if w is not None:
                deps.add(w)
            for rd in self.res_r.get(k, ()):
                deps.add(rd)
        for r in reads:
            self.res_r.setdefault(r, []).append(idx)
        for k in writes:
            self.res_w[k] = idx
            self.res_r[k] = []
        deps.discard(idx)
        chan = writes[0] if dma else None
        self.ops.append(dict(eng=eng, fn=fn, deps=deps, dma=dma, chan=chan))
        if dma:
            self.last_chan[chan] = idx
        elif fn is not None:
            self.last_eng[eng] = idx
        return idx

    def barrier(self):
        deps = list(self.last_eng.values()) + list(self.last_chan.values())
        for e in self.ENGS:
            self.op(e, None, extra_deps=deps)
        self.res_w.clear()
        self.res_r.clear()

    def finalize(self, nc, stack):
        ops = self.ops

        def skip(p, o):
            return (not p["dma"]) and (not o["dma"]) and p["eng"] == "pe" and o["eng"] == "pe" \
                and p["fn"] is not None and o["fn"] is not None

        need = [False] * len(ops)
        for o in ops:
            for d in o["deps"]:
                p = ops[d]
                if p["dma"] or skip(p, o):
                    continue
                need[d] = True
        self.esem = {e: stack.enter_context(nc.semaphore("s_" + e)) for e in self.ENGS}
        chans = []
        seen = set()
        for o in ops:
            if o["dma"] and o["chan"] not in seen:
                seen.add(o["chan"])
                chans.append(o["chan"])
        self.csem = {c: stack.enter_context(nc.semaphore("d%d" % i)) for i, c in enumerate(chans)}
        ecount = {e: 0 for e in self.ENGS}
        ccount = {c: 0 for c in chans}
        for i, o in enumerate(ops):
            if o["dma"]:
                ccount[o["chan"]] += 16
                o["sig"] = (self.csem[o["chan"]], 16, ccount[o["chan"]])
            elif need[i]:
                ecount[o["eng"]] += 1
                o["sig"] = (self.esem[o["eng"]], 1, ecount[o["eng"]])
            else:
                o["sig"] = None
        waited = {e: {} for e in self.ENGS}
        for o in ops:
            ws = {}
            for d in o["deps"]:
                p = ops[d]
                if skip(p, o):
                    continue
                sem, _, val = p["sig"]
                key = id(sem)
                if key not in ws or ws[key][1] < val:
                    ws[key] = (sem, val)
            out = []
            for key, (sem, val) in ws.items():
                if waited[o["eng"]].get(key, 0) >= val:
                    continue
                waited[o["eng"]][key] = val
                out.append((sem, val))
            o["waits"] = out
        self.nsem = len(chans) + len(self.ENGS)

    def emit(self, ename, eng):
        for o in self.ops:
            if o["eng"] != ename:
                continue
            for sem, val in o["waits"]:
                eng.wait_ge(sem, val)
            if o["fn"] is None:
                continue
            ins = o["fn"](eng)
            if o["sig"] is not None:
                ins.then_inc(o["sig"][0], o["sig"][1])


class Plan:
    def __init__(self):
        self.items = []

    def add(self, name, shape, dtype, p0, p1):
        nbytes = int(np.prod(shape[1:])) * (4 if dtype == F32 else 2)
        nbytes = (nbytes + 63) // 64 * 64
        self.items.append(dict(name=name, shape=list(shape), dtype=dtype, p0=p0, p1=p1, n=nbytes))

    def place(self, nc):
        for ph in sorted(set([i["p0"] for i in self.items] + [i["p1"] for i in self.items])):
            tot = sum(i["n"] for i in self.items if i["p0"] <= ph <= i["p1"])
            print("phase", ph, "sbuf bytes", tot)
        placed = []
        out = {}
        for it in sorted(self.items, key=lambda t: -t["n"]):
            conf = sorted([(q["off"], q["off"] + q["n"]) for q in placed
                           if not (q["p1"] < it["p0"] or it["p1"] < q["p0"])])
            off = SBUF_BASE
            for a, b in conf:
                if off + it["n"] <= a:
                    break
                off = max(off, b)
            it["off"] = off
            assert off + it["n"] <= SBUF_LIMIT, ("SBUF overflow", it["name"], off + it["n"])
            placed.append(it)
            out[it["name"]] = nc.alloc_sbuf_tensor_at(it["name"], it["shape"], it["dtype"], offset=off)
        return out


def bc_last(ap2d, n):
    a = ap2d.ap
    return bass.AP(ap2d.tensor, ap2d.offset, [list(a[0]), list(a[1]), [0, n]])


def bc_mid(ap2d, n):
    a = ap2d.ap
    return bass.AP(ap2d.tensor, ap2d.offset, [list(a[0]), [0, n], list(a[1])])


def build_program(debug=None):
    nc = bass.Bass("TRN2", target_bir_lowering=False)
    P = Prog()

    def din(name, shape, dt=F32):
        return nc.dram_tensor(name, list(shape), dt, kind="ExternalInput").ap()

    xk = din("xk", [NKEY, D])
    xhalo = din("xhalo", [4, 16, D])
    hmask = din("hmask", [128, 4, 16])
    icntc = din("icntc", [16, 512])
    cosk = din("cosk", [32, NKEY])
    sink = din("sink", [32, NKEY])
    cvec = din("cvec", [128, 8, 2])
    ident_d = din("ident", [128, 128])
    w_ada = din("w_ada", [D, 6 * D])
    b_ada_fm = din("b_ada_fm", [128, 48])
    b_ada_row = din("b_ada_row", [1, 6 * D])
    w_in = din("w_in", [D, 1312])
    qg_fm = din("qg_fm", [128, 4])
    kvg_fm = din("kvg_fm", [128, 2])
    w_uq = din("w_uq", [512, 768])
    w_ukv = din("w_ukv", [256, 1024])
    w_pool = din("w_pool", [4, 128, 128])
    pscale_fm = din("pscale_fm", [128, 4])
    w_out = din("w_out", [D, D])
    ln1_g = din("ln1_g", [1, D])
    ln1_b = din("ln1_b", [1, D])
    ln2_g = din("ln2_g", [1, D])
    ln2_b = din("ln2_b", [1, D])
    w_router = din("w_router", [D, NEXP])
    rbias = din("rbias", [1, NEXP])
    w_eg = din("w_eg", [NEXP + 1, D, 256])
    w_eu = din("w_eu", [NEXP + 1, D, 256])
    w_ed = din("w_ed", [NEXP + 1, 256, D])
    yout = nc.dram_tensor("yout", [TOWN, D], F32, kind="ExternalOutput").ap()
    dbg_out = {}
    if debug:
        for name, shape in debug.items():
            dbg_out[name] = nc.dram_tensor("dbg_" + name, list(shape), F32, kind="ExternalOutput").ap()

    pl = Plan()
    A = pl.add
    A("ident", [128, 128], F32, 0, 6)
    A("onesb", [128, 128], BF16, 0, 6)
    A("modT", [128, 96], F32, 0, 6)
    A("mods", [128, 64], F32, 0, 6)
    A("g1bc", [128, D], F32, 0, 3)
    A("g2bc", [128, D], F32, 0, 5)
    A("stat", [128, 16, 8], F32, 0, 6)
    A("wada0", [128, 8, 512], F32, 0, 0)
    A("wada1", [128, 8, 512], F32, 0, 0)
    A("wada2", [128, 8, 512], F32, 0, 0)
    A("cT", [128, 8, 2], F32, 0, 0)
    A("scT", [128, 8, 2], F32, 0, 0)
    A("modrow", [2, 6 * D], F32, 0, 0)
    A("brow", [2, 6 * D], F32, 0, 0)
    A("sel2", [2, 128], F32, 0, 0)
    A("kvnT", [128, 2, NKEY], BF16, 0.5, 2)
    A("KT", [128, NKEY], BF16, 1, 2)
    A("qnT", [128, 4, TOWN], BF16, 1, 2)
    A("pooledT", [128, 4, TOWN], BF16, 1, 3)
    A("WUKV", [128, 2, 1024], BF16, 0.5, 2)
    A("WUQ", [128, 4, 768], BF16, 0.5, 2)
    A("WUQR", [128, 4, 256], BF16, 0.5, 2)
    A("WIN", [128, 8, 1312], BF16, 0.5, 1)
    A("WKR32", [128, 8, 32], F32, 0.5, 0.5)
    A("WKR", [128, 8, 32], BF16, 0.5, 1)
    A("WPOOL", [128, 4, 128], BF16, 0.5, 1)
    A("stg32", [128, 4, 768], F32, 0.5, 0.5)
    A("qg", [128, 4], F32, 0.5, 0.5)
    A("kvg", [128, 2], F32, 0.5, 0.5)
    A("psc", [128, 4], F32, 0.5, 1)
    for i in range(4):
        A("xt%d" % i, [128, D], F32, 1, 1)
    A("xh", [16, D], F32, 1, 1)
    A("hT0", [128, 8, 512], BF16, 1, 1)
    A("hT1", [128, 8, 512], BF16, 1, 1)
    A("hTh", [128, 8, 16], BF16, 1, 1)
    A("sq", [128, 4, 512], BF16, 1, 1)
    A("rbc", [128, 512], F32, 1, 1)
    A("latsb", [128, 2, 512], F32, 1, 1)
    A("cs0", [128, 2, 512], F32, 1, 2)
    A("cs1", [128, 2, 512], F32, 1, 2)
    A("rt1", [128, 512], F32, 1, 2)
    A("rt2", [128, 512], F32, 1, 2)
    A("uext", [128, 4, 528], F32, 1, 1)
    A("ic0", [128, 512], F32, 1, 1)
    A("pa", [128, 528], F32, 1, 1)
    A("pb", [128, 528], F32, 1, 1)
    A("ic1", [128, 512], F32, 1, 1)
    A("pin", [128, 4, 512], BF16, 1, 1)
    A("hm", [128, 4, 16], F32, 0.5, 1)
    A("attnT", [64, 8, TOWN], BF16, 2, 3)
    A("VH0", [128, 66, 65], BF16, 2, 2)
    A("VH1", [128, 66, 65], BF16, 2, 2)
    A("QH", [128, TOWN], BF16, 2, 2)
    for i in range(3):
        A("PTP%d" % i, [128, 1024], BF16, 2, 2)
    A("rden", [128, 512], F32, 2, 2)
    A("num", [64, 512], F32, 2, 2)
    A("c1row", [128, 64], F32, 2, 2)
    A("WOA", [64, 8, D], BF16, 3, 3)
    A("WOP", [128, 4, D], BF16, 3, 3)
    A("acc", [128, 16, D], F32, 3, 6)
    for i in range(4):
        A("xs%d" % i, [128, D], F32, 3, 3)
    A("l1g", [128, D], F32, 3, 3)
    A("l1b", [128, D], F32, 3, 3)
    A("h2T", [128, 8, TOWN], BF16, 4, 5)
    A("h2f0", [128, 8, 128], F32, 4, 4)
    A("h2f1", [128, 8, 128], F32, 4, 4)
    A("wr", [128, 8, NEXP], F32, 4, 4)
    A("rb", [128, NEXP], F32, 4, 4)
    A("scores", [128, 16, NEXP], F32, 4, 4)
    A("gates", [128, 16, NEXP], F32, 4, 5)
    A("ra", [128, 16, NEXP], F32, 4, 4)
    A("rbb", [128, 16, NEXP], F32, 4, 4)
    A("rc", [128, 16, NEXP], F32, 4, 4)
    A("rs", [128, 16, 8], F32, 4, 4)
    A("rs2", [128, 16, 8], F32, 4, 4)
    A("rs3", [128, 16, 8], F32, 4, 4)
    A("rs4", [128, 16, 8], F32, 4, 4)
    A("t8", [128, 16, 8], F32, 4, 4)
    A("rsum", [128, 16], F32, 4, 4)
    for i in range(2):
        A("wg%d" % i, [128, 8, 256], BF16, 4, 5)
        A("wu%d" % i, [128, 8, 256], BF16, 4, 5)
        A("wd32_%d" % i, [128, 2, D], F32, 4, 5)
        A("wd%d" % i, [128, 2, D], BF16, 4, 5)
        A("sg%d" % i, [128, 512], F32, 5, 5)
        A("aT%d" % i, [128, 2, 512], BF16, 5, 5)
    A("l2g", [128, D], F32, 5, 6)
    A("l2b", [128, D], F32, 5, 6)
    for i in range(4):
        A("o%d" % i, [128, D], F32, 5, 6)
    T = pl.place(nc)

    st = ExitStack()
    PSP = [st.enter_context(nc.psum_tensor("psp%d" % i, [128, 1024], F32)) for i in range(4)]
    PS = [PSP[i // 2][:, (i % 2) * 512:(i % 2 + 1) * 512] for i in range(8)]

    def B(i):
        return ("B", i)

    def dma(q, out, in_, reads=(), writes=(), nonc=False):
        if nonc:
            def fn(e):
                with nc.allow_non_contiguous_dma(reason="small strided constant load"):
                    return e.dma_start(out=out, in_=in_)
        else:
            def fn(e):
                return e.dma_start(out=out, in_=in_)
        return P.op(q, fn, reads=list(reads), writes=list(writes), dma=True)

    def ln_tile(src_ap, dst_ap, gbc, bbc, skey, slot, rsrc, wdst, extra_reads=()):
        s = T["stat"][:, slot, :]
        skey = (skey, slot)
        P.op("act", lambda e: e.activation(out=dst_ap, in_=src_ap, func=AF.Identity, accum_out=s[:, 0:1]),
             reads=[rsrc, "statz"] + list(extra_reads), writes=[wdst, (skey, 0)])
        P.op("act", lambda e: e.activation(out=dst_ap, in_=src_ap, func=AF.Square, accum_out=s[:, 1:2]),
             reads=[rsrc, "statz"], writes=[wdst, (skey, 1)])
        P.op("dve", lambda e: e.tensor_scalar(out=s[:, 2:3], in0=s[:, 0:1], scalar1=1.0 / D, scalar2=None, op0=ALU.mult),
             reads=[(skey, 0)], writes=[(skey, 2)])
        P.op("dve", lambda e: e.tensor_tensor(out=s[:, 3:4], in0=s[:, 2:3], in1=s[:, 2:3], op=ALU.mult),
             reads=[(skey, 2)], writes=[(skey, 3)])
        P.op("dve", lambda e: e.scalar_tensor_tensor(out=s[:, 4:5], in0=s[:, 1:2], scalar=1.0 / D, in1=s[:, 3:4],
                                                     op0=ALU.mult, op1=ALU.subtract),
             reads=[(skey, 1), (skey, 3)], writes=[(skey, 4)])
        P.op("dve", lambda e: e.tensor_scalar(out=s[:, 4:5], in0=s[:, 4:5], scalar1=LN_EPS, scalar2=None, op0=ALU.add),
             reads=[(skey, 4)], writes=[(skey, 4)])
        P.op("act", lambda e: e.activation(out=s[:, 7:8], in_=s[:, 4:5], func=AF.Sqrt),
             reads=[(skey, 4)], writes=[(skey, 7)])
        P.op("dve", lambda e: e.reciprocal(out=s[:, 5:6], in_=s[:, 7:8]),
             reads=[(skey, 7)], writes=[(skey, 5)])
        P.op("dve", lambda e: e.scalar_tensor_tensor(out=s[:, 6:7], in0=s[:, 2:3], scalar=-1.0, in1=s[:, 5:6],
                                                     op0=ALU.mult, op1=ALU.mult),
             reads=[(skey, 2), (skey, 5)], writes=[(skey, 6)])
        P.op("act", lambda e: e.activation(out=dst_ap, in_=src_ap, func=AF.Identity, bias=s[:, 6:7], scale=s[:, 5:6]),
             reads=[rsrc, (skey, 5), (skey, 6), (skey, 1)], writes=[wdst])
        P.op("dve", lambda e: e.tensor_tensor(out=dst_ap, in0=dst_ap, in1=gbc, op=ALU.mult),
             reads=[wdst], writes=[wdst])
        P.op("dve", lambda e: e.tensor_tensor(out=dst_ap, in0=dst_ap, in1=bbc, op=ALU.add),
             reads=[wdst], writes=[wdst])

    def dump(name, ap_sb, reads):
        if debug and name in debug:
            dma("pool", dbg_out[name], ap_sb, reads=reads, writes=[("dbg", name)])

    idn, onesb, modT, mods = T["ident"], T["onesb"], T["modT"], T["mods"]
    modrow, sel2 = T["modrow"], T["sel2"]
    dma("sp", idn[:], ident_d, writes=["ident"])
    dma("sp", T["cT"][:], cvec, writes=["cT"])
    dma("sp", T["brow"][:], b_ada_row.partition_broadcast(2), writes=["brow"])
    P.op("pool", lambda e: e.memset(onesb[:], 1.0), writes=["onesb"])
    P.op("pool", lambda e: e.memset(sel2[:], 0.0), writes=["sel2"])
    P.op("pool", lambda e: e.memset(sel2[0:1, :], 1.0), reads=["sel2"], writes=["sel2"])
    P.op("act", lambda e: e.activation(out=T["scT"][:], in_=T["cT"][:], func=AF.Silu), reads=["cT"], writes=["scT"])
    wada_v = w_ada.rearrange("(j p) n -> p j n", p=128)
    for cb in range(12):
        wb = T["wada%d" % (cb % 3)]
        wk = "wada%d" % (cb % 3)
        dma("sp" if cb % 2 == 0 else "act", wb[:], wada_v[:, :, cb * 512:(cb + 1) * 512], writes=[wk])
        bank = cb % 2

        def fn(e, wb=wb, bank=bank):
            for j in range(8):
                ins = e.matmul(PS[bank][0:2, :], lhsT=T["scT"][:, j, :], rhs=wb[:, j, :], start=(j == 0), stop=(j == 7))
            return ins
        P.op("pe", fn, reads=[wk, "scT"], writes=[B(bank)])
        csl = slice(cb * 512, (cb + 1) * 512)
        P.op("dve", lambda e, bank=bank, csl=csl: e.tensor_tensor(out=modrow[:, csl], in0=PS[bank][0:2, :], in1=T["brow"][:, csl], op=ALU.add),
             reads=[B(bank), "brow"], writes=[("modrow", cb)])
    def fnT(e):
        for ch in range(48):
            ins = e.transpose(out=PS[2][:, ch * 2:ch * 2 + 2], in_=modrow[:, ch * 128:(ch + 1) * 128], identity=idn[0:2, 0:2])
        return ins
    P.op("pe", fnT, reads=["ident"] + [("modrow", cb) for cb in range(12)], writes=[B(2)])
    P.op("dve", lambda e: e.tensor_copy(out=modT[:], in_=PS[2][:, 0:96]), reads=[B(2)], writes=["modT"])
    for gi, (gname, c0) in enumerate((("g1bc", 2 * D), ("g2bc", 5 * D))):
        for half in range(2):
            bank = 3 + (gi * 2 + half) % 2
            P.op("pe", lambda e, bank=bank, c0=c0, half=half: e.matmul(
                PS[bank][:, :], lhsT=sel2[:, :], rhs=modrow[:, c0 + half * 512:c0 + (half + 1) * 512], start=True, stop=True),
                reads=["sel2"] + [("modrow", cb) for cb in range(12)], writes=[B(bank)])
            P.op("act", lambda e, bank=bank, gname=gname, half=half: e.activation(
                out=T[gname][:, half * 512:(half + 1) * 512], in_=PS[bank][:, :], func=AF.Identity),
                reads=[B(bank)], writes=[(gname, half)])
    modv = modT[:].rearrange("p (c t) -> p c t", t=2)
    P.op("dve", lambda e: e.tensor_scalar(out=mods[:, 0:8], in0=modv[:, 8:16, 0], scalar1=1.0, scalar2=None, op0=ALU.add),
         reads=["modT"], writes=[("mods", 0)])
    P.op("dve", lambda e: e.tensor_scalar(out=mods[:, 8:16], in0=modv[:, 8:16, 1], scalar1=1.0, scalar2=None, op0=ALU.add),
         reads=["modT"], writes=[("mods", 1)])
    P.op("dve", lambda e: e.tensor_scalar(out=mods[:, 16:24], in0=modv[:, 32:40, 0], scalar1=1.0, scalar2=1.0 / ALPHA,
                                          op0=ALU.add, op1=ALU.mult),
         reads=["modT"], writes=[("mods", 2)])
    P.barrier()

    WIN, WKR, WPOOL = T["WIN"], T["WKR"], T["WPOOL"]
    kvnT, KT, qnT, pooledT = T["kvnT"], T["KT"], T["qnT"], T["pooledT"]
    dma("pool", WIN[:], w_in.rearrange("(j p) n -> p j n", p=128), writes=["WIN"])
    dma("pool", WPOOL[:], w_pool.rearrange("g c d -> c g d"), writes=["WPOOL"])
    dma("sp", T["qg"][:], qg_fm, writes=["qg"])
    dma("sp", T["kvg"][:], kvg_fm, writes=["kvg"])
    dma("sp", T["psc"][:], pscale_fm, writes=["psc"])
    dma("sp", T["hm"][:], hmask, writes=["hm"])
    w_in_v = w_in.rearrange("(j p) n -> p j n", p=128)
    srcblk = [8, 0, 24, 16]
    for q in range(4):
        dma("sp", T["WKR32"][:, :, q * 8:(q + 1) * 8], w_in_v[:, :, 768 + srcblk[q]:768 + srcblk[q] + 8],
            writes=[("WKR32", q)], nonc=True)
    wkr32v = T["WKR32"][:].rearrange("p j (q k) -> p j q k", k=8)
    wkrv = WKR[:].rearrange("p j (q k) -> p j q k", k=8)
    for q in range(4):
        sgn = -1.0 if q % 2 == 0 else 1.0
        P.op("dve", lambda e, q=q, sgn=sgn: e.tensor_scalar(
            out=WKR[:, :, q * 8:(q + 1) * 8], in0=T["WKR32"][:, :, q * 8:(q + 1) * 8], scalar1=sgn, scalar2=None, op0=ALU.mult),
            reads=[("WKR32", q)], writes=[("WKR", q)])
    stg = T["stg32"]
    stg_kv = stg[:, 0:2, :]
    for half in range(2):
        dma("sp", stg[:, 0:2, 0:512], w_ukv.rearrange("(m p) n -> p m n", p=128)[:, :, half * 512:(half + 1) * 512],
            writes=["stg"])
        for m in range(2):
            P.op("dve", lambda e, m=m, half=half: e.tensor_scalar(
                out=T["WUKV"][:, m, half * 512:(half + 1) * 512], in0=stg[:, m, 0:512],
                scalar1=T["kvg"][:, m:m + 1], scalar2=None, op0=ALU.mult),
                reads=["stg", "kvg"], writes=[("WUKV", m, half)])
    dma("sp", stg[:], w_uq.rearrange("(m p) n -> p m n", p=128), reads=[("WUKV", 1, 1)], writes=["stg"])
    for m in range(4):
        P.op("dve", lambda e, m=m: e.tensor_scalar(out=T["WUQ"][:, m, :], in0=stg[:, m, :], scalar1=T["qg"][:, m:m + 1],
                                                   scalar2=None, op0=ALU.mult),
             reads=["stg", "qg"], writes=[("WUQ", m)])
        sv = stg[:, m, :].rearrange("p (h c) -> p h c", c=96)
        dv = T["WUQR"][:, m, :].rearrange("p (h c) -> p h c", c=32)
        for q in range(4):
            sgn = -1.0 if q % 2 == 0 else 1.0
            P.op("dve", lambda e, m=m, q=q, sgn=sgn, sv=sv, dv=dv: e.tensor_scalar(
                out=dv[:, :, q * 8:(q + 1) * 8], in0=sv[:, :, 64 + srcblk[q]:64 + srcblk[q] + 8],
                scalar1=T["qg"][:, m:m + 1], scalar2=sgn, op0=ALU.mult, op1=ALU.mult),
                reads=["stg", "qg"], writes=[("WUQR", m, q)])
    P.barrier()
    wuq_keys = [("WUQ", m) for m in range(4)]
    wuqr_keys = [("WUQR", m, q) for m in range(4) for q in range(4)]
    wukv_keys = [("WUKV", m, h) for m in range(2) for h in range(2)]

    xk_t = xk.rearrange("(t p) d -> t p d", p=128)
    xt_i = [0]

    def load_cs(blk, nt, buf):
        cs = T["cs%d" % buf]
        k = "cs%d" % buf
        dma("sp", cs[64:96, 0, 0:nt], cosk[:, blk * 512:blk * 512 + nt], writes=[k])
        dma("sp", cs[64:96, 1, 0:nt], sink[:, blk * 512:blk * 512 + nt], writes=[k])
        return cs, k

    def make_hT(blk, ntile, hT, hk, col):
        xts = []
        for t in range(ntile):
            i = xt_i[0] % 4
            xt_i[0] += 1
            dma("sp", T["xt%d" % i][:], xk_t[blk * 4 + t], writes=["xt%d" % i])
            xts.append(i)
        for j in range(8):
            bank = j % 2

            def fn(e, j=j, bank=bank):
                for t, i in enumerate(xts):
                    ins = e.transpose(out=PS[bank][:, t * 128:(t + 1) * 128], in_=T["xt%d" % i][:, j * 128:(j + 1) * 128],
                                      identity=idn[:])
                return ins
            P.op("pe", fn, reads=["ident"] + ["xt%d" % i for i in xts], writes=[B(bank)])
            P.op("act", lambda e, j=j, bank=bank: e.activation(
                out=hT[:, j, 0:ntile * 128], in_=PS[bank][:, 0:ntile * 128], func=AF.Identity,
                bias=modv[:, j, col:col + 1], scale=mods[:, col * 8 + j:col * 8 + j + 1]),
                reads=[B(bank), "modT", ("mods", col)], writes=[(hk, j)])

    def proj(hT, hk, nt, c0, M, bank, prow=0):
        def fn(e):
            for j in range(8):
                ins = e.matmul(PS[bank][prow:prow + M, 0:nt], lhsT=WIN[:, j, c0:c0 + M], rhs=hT[:, j, 0:nt],
                               start=(j == 0), stop=(j == 7))
            return ins
        P.op("pe", fn, reads=["WIN"] + [(hk, j) for j in range(8)], writes=[B(bank)])

    def rms_bcast(banks, nt, inv_n, sqk):
        sq = T["sq"]
        for i, bk in enumerate(banks):
            P.op("act", lambda e, i=i, bk=bk: e.activation(out=sq[:, i, 0:nt], in_=PS[bk][:, 0:nt], func=AF.Square),
                 reads=[B(bk)], writes=[("sq", i)])

        def fn(e):
            for i in range(len(banks)):
                ins = e.matmul(PS[6][:, 0:nt], lhsT=onesb[:], rhs=sq[:, i, 0:nt], start=(i == 0), stop=(i == len(banks) - 1))
            return ins
        P.op("pe", fn, reads=["onesb"] + [("sq", i) for i in range(len(banks))], writes=[B(6)])
        P.op("dve", lambda e: e.tensor_scalar(out=T["rbc"][:, 0:nt], in0=PS[6][:, 0:nt], scalar1=inv_n, scalar2=RMS_EPS,
                                              op0=ALU.mult, op1=ALU.add),
             reads=[B(6)], writes=["rbc"])
        P.op("act", lambda e: e.activation(out=T["rbc"][:, 0:nt], in_=T["rbc"][:, 0:nt], func=AF.Sqrt),
             reads=["rbc"], writes=["rbc"])
        P.op("dve", lambda e: e.reciprocal(out=T["rbc"][:, 0:nt], in_=T["rbc"][:, 0:nt]),
             reads=["rbc"], writes=["rbc"])

    def rope_combine(bankA, bankB, cs, csk, nt, dst_ap, dkey, eng="dve"):
        r1, r2 = T["rt1"], T["rt2"]
        P.op("dve", lambda e: e.tensor_tensor(out=r1[64:96, 0:nt], in0=PS[bankA][64:96, 0:nt], in1=cs[64:96, 0, 0:nt], op=ALU.mult),
             reads=[B(bankA), csk], writes=["rt1"])
        P.op("dve", lambda e: e.tensor_tensor(out=r2[64:96, 0:nt], in0=PS[bankB][64:96, 0:nt], in1=cs[64:96, 1, 0:nt], op=ALU.mult),
             reads=[B(bankB), csk], writes=["rt2"])
        P.op("pool", lambda e: e.tensor_tensor(out=dst_ap, in0=r1[64:96, 0:nt], in1=r2[64:96, 0:nt], op=ALU.add),
             reads=["rt1", "rt2"], writes=[dkey])

    pre = {}
    deferred = []

    order = [0, 4, 5, 6, 1, 7, 8, 9, 2, 10, 11, 12, 3, 13, 14, 15, 16]

    def stageA(pos):
        blk = order[pos]
        nt = 512 if blk < 16 else 256
        cs, csk = load_cs(blk, nt, pos % 2)
        make_hT(blk, nt // 128, T["hT%d" % (pos % 2)], "hT%d" % (pos % 2), 0 if blk < 16 else 1)
        pre[pos] = (cs, csk)
    stageA(0)
    for pos in range(17):
        blk = order[pos]
        own = blk < 4
        nt = 512 if blk < 16 else 256
        ntile = nt // 128
        col = 0 if blk < 16 else 1
        hT = T["hT%d" % (pos % 2)]
        hk = "hT%d" % (pos % 2)
        cs, csk = pre[pos]
        ksl = slice(blk * 512, blk * 512 + nt)
        proj(hT, hk, nt, 512, 128, 2)
        proj(hT, hk, nt, 640, 128, 3)
        proj(hT, hk, nt, 768, 32, 4, prow=64)

        def fnB(e, hT=hT, nt=nt):
            for j in range(8):
                ins = e.matmul(PS[5][64:96, 0:nt], lhsT=WKR[:, j, :], rhs=hT[:, j, 0:nt], start=(j == 0), stop=(j == 7))
            return ins
        P.op("pe", fnB, reads=[("WKR", q) for q in range(4)] + [(hk, j) for j in range(8)], writes=[B(5)])
        for m in range(2):
            P.op("act", lambda e, m=m, nt=nt: e.activation(out=T["latsb"][:, m, 0:nt], in_=PS[2 + m][:, 0:nt], func=AF.Identity),
                 reads=[B(2 + m)], writes=[("latsb", m)])
        rms_bcast([2, 3], nt, 1.0 / 256, "sq")
        if pos + 1 < 17:
            stageA(pos + 1)
        for m in range(2):
            P.op("dve", lambda e, m=m, ksl=ksl, nt=nt: e.tensor_tensor(out=kvnT[:, m, ksl], in0=T["latsb"][:, m, 0:nt],
                                                                      in1=T["rbc"][:, 0:nt], op=ALU.mult),
                 reads=[("latsb", m), "rbc"], writes=[("kvnT", m, blk)])
        rope_combine(4, 5, cs, csk, nt, KT[64:96, ksl], ("KTr", blk))
        deferred_new = []
        if not own and pos % 4 == 3:
            for f_ in deferred:
                f_()
            deferred.clear()
        if own:
            qsl = slice(blk * 512, (blk + 1) * 512)
            for m in range(4):
                proj(hT, hk, 512, m * 128, 128, 2 + m)
            rms_bcast([2, 3, 4, 5], 512, 1.0 / 512, "sq")
            for m in range(4):
                P.op("dve", lambda e, m=m, qsl=qsl: e.tensor_tensor(out=qnT[:, m, qsl], in0=PS[2 + m][:, :],
                                                                    in1=T["rbc"][:, :], op=ALU.mult),
                     reads=[B(2 + m), "rbc"], writes=[("qnT", m, blk)])
            uext, pa, pb = T["uext"], T["pa"], T["pb"]
            dma("sp", T["xh"][:], xhalo[blk], writes=["xh"])
            for j in range(8):
                bank = j % 2
                P.op("pe", lambda e, j=j, bank=bank: e.transpose(out=PS[bank][:, 0:16], in_=T["xh"][:, j * 128:(j + 1) * 128],
                                                                identity=idn[0:16, 0:16]),
                     reads=["ident", "xh"], writes=[B(bank)])
                P.op("act", lambda e, j=j, bank=bank: e.activation(
                    out=T["hTh"][:, j, :], in_=PS[bank][:, 0:16], func=AF.Identity,
                    bias=modv[:, j, 0:1], scale=mods[:, j:j + 1]),
                    reads=[B(bank), "modT", ("mods", 0)], writes=[("hTh", j)])
            for g in range(4):
                bank = 2 + g
                proj(hT, hk, 512, 800 + g * 128, 128, bank)
                P.op("act", lambda e, g=g, bank=bank: e.activation(out=uext[:, g, 8:520], in_=PS[bank][:, :], func=AF.Identity),
                     reads=[B(bank)], writes=[("uext", g, 1)])
            for g in range(4):
                def fn(e, g=g):
                    for j in range(8):
                        ins = e.matmul(PS[7][:, g * 16:(g + 1) * 16], lhsT=WIN[:, j, 800 + g * 128:800 + (g + 1) * 128],
                                       rhs=T["hTh"][:, j, :], start=(j == 0), stop=(j == 7))
                    return ins
                P.op("pe", fn, reads=["WIN"] + [("hTh", j) for j in range(8)], writes=[B(7)])
            p7 = PS[7][:, 0:64].rearrange("p (g k) -> p g k", k=16)
            P.op("dve", lambda e, blk=blk: e.tensor_tensor(out=uext[:, :, 0:8], in0=p7[:, :, 0:8],
                                                           in1=bc_mid(T["hm"][:, blk, 0:8], 4), op=ALU.mult),
                 reads=[B(7), "hm"], writes=[("uext", 0, 0), ("uext", 1, 0), ("uext", 2, 0), ("uext", 3, 0)])
            P.op("dve", lambda e, blk=blk: e.tensor_tensor(out=uext[:, :, 520:528], in0=p7[:, :, 8:16],
                                                           in1=bc_mid(T["hm"][:, blk, 8:16], 4), op=ALU.mult),
                 reads=[B(7), "hm"], writes=[("uext", 0, 2), ("uext", 1, 2), ("uext", 2, 2), ("uext", 3, 2)])
            for f_ in deferred:
                f_()
            deferred.clear()
            def chain(src_ap_fn, src_keys, g, tag):
                cur, ckeys, ln, step = src_ap_fn, src_keys, 528, 1
                bufs = [pa, pb]
                bkeys = ["pa", "pb"]
                k = 0
                for lvl in range(g + 1):
                    dst = bufs[k % 2]
                    dk = bkeys[k % 2]
                    ln2 = ln - step
                    P.op("pool", lambda e, cur=cur, dst=dst, ln2=ln2, step=step: e.tensor_tensor(
                        out=dst[:, 0:ln2], in0=cur(0, ln2), in1=cur(step, step + ln2), op=ALU.add),
                        reads=list(ckeys), writes=[dk])
                    cur = (lambda dst: (lambda a, b: dst[:, a:b]))(dst)
                    ckeys = [dk]
                    ln, step = ln2, step * 2
                    k += 1
                return cur, ckeys
            for g in range(4):
                half = 1 << g
                o = 8 - half
                ic = T["ic%d" % (g % 2)]
                ick = "ic%d" % (g % 2)
                dma("sp", ic[:], icntc[blk * 4 + g:blk * 4 + g + 1, :].partition_broadcast(128), writes=[ick])
                cur, ck = chain(lambda a, b, g=g: uext[:, g, a:b], [("uext", g, i) for i in range(3)], g, "u")
                P.op("dve", lambda e, g=g, cur=cur, o=o, ic=ic: e.tensor_tensor(out=ic[:], in0=cur(o, o + 512), in1=ic[:], op=ALU.mult),
                     reads=list(ck) + [ick], writes=[ick])
                P.op("dve", lambda e, g=g, ic=ic: e.tensor_tensor(out=T["pin"][:, g, :], in0=ic[:], in1=uext[:, g, 8:520],
                                                                  op=ALU.subtract),
                     reads=[ick, ("uext", g, 1)], writes=[("pin", g)])
                bank = 2 + g

                def later(g=g, bank=bank, qsl=qsl, blk=blk):
                    P.op("pe", lambda e: e.matmul(PS[bank][:, :], lhsT=WPOOL[:, g, :], rhs=T["pin"][:, g, :], start=True, stop=True),
                         reads=["WPOOL", ("pin", g)], writes=[B(bank)])
                    P.op("act", lambda e: e.activation(out=pooledT[:, g, qsl], in_=PS[bank][:, :],
                                                       func=AF.Identity, scale=T["psc"][:, g:g + 1]),
                         reads=[B(bank), "psc"], writes=[("pooledT", g, blk)])
                deferred_new.append(later)
            deferred.extend(deferred_new)
    for f_ in deferred:
        f_()
    deferred.clear()
    dump("kvnT", kvnT[:, 0, :], reads=[("kvnT", 0, b_) for b_ in range(17)])
    P.barrier()

    attnT, QH = T["attnT"], T["QH"]
    WUKV, WUQ, WUQR = T["WUKV"], T["WUQ"], T["WUQR"]
    P.op("pool", lambda e: e.memset(T["c1row"][:], 1.0), writes=["c1row"])
    for i in range(2):
        P.op("pool", lambda e, i=i: e.memset(T["VH%d" % i][:, :, 64:65], 1.0), writes=[("VH1s", i)])
    pt_i = [0]
    s_i = [0]
    o_i = [0]
    for h in range(NH):
        VH = T["VH%d" % (h % 2)]
        vk = ("VH", h % 2)
        for blk in range(17):
            nt = 512 if blk < 16 else 256
            ksl = slice(blk * 512, blk * 512 + nt)
            bank = 4 + blk % 2

            def fn(e, ksl=ksl, nt=nt, bank=bank, h=h):
                for m in range(2):
                    ins = e.matmul(PS[bank][0:64, 0:nt], lhsT=WUKV[:, m, h * 128:h * 128 + 64], rhs=kvnT[:, m, ksl],
                                   start=(m == 0), stop=(m == 1))
                return ins
            P.op("pe", fn, reads=[], writes=[B(bank)])
            eng = "act" if blk % 2 == 0 else "dve"
            if eng == "act":
                P.op("act", lambda e, ksl=ksl, nt=nt, bank=bank: e.activation(out=KT[0:64, ksl], in_=PS[bank][0:64, 0:nt], func=AF.Identity),
                     reads=[B(bank)], writes=[("KTn", blk)])
            else:
                P.op("dve", lambda e, ksl=ksl, nt=nt, bank=bank: e.tensor_copy(out=KT[0:64, ksl], in_=PS[bank][0:64, 0:nt]),
                     reads=[B(bank)], writes=[("KTn", blk)])
        for c0 in range(0, 66, 8):
            ncz = min(8, 66 - c0)
            bank = 4 + (c0 // 8) % 2

            def fn(e, c0=c0, ncz=ncz, bank=bank, h=h):
                for cc in range(ncz):
                    c = c0 + cc
                    for m in range(2):
                        ins = e.matmul(PS[bank][:, cc * 64:(cc + 1) * 64], lhsT=kvnT[:, m, c * 128:(c + 1) * 128],
                                       rhs=WUKV[:, m, h * 128 + 64:h * 128 + 128], start=(m == 0), stop=(m == 1))
                return ins
            P.op("pe", fn, reads=[], writes=[B(bank)])
            P.op("dve", lambda e, c0=c0, ncz=ncz, bank=bank, VH=VH: e.tensor_copy(
                out=VH[:, c0:c0 + ncz, 0:64], in_=PS[bank][:, 0:ncz * 64].rearrange("p (c v) -> p c v", v=64)),
                reads=[B(bank)], writes=[(vk, c0 // 8)])
        for qb in range(4):
            qsl = slice(qb * 512, (qb + 1) * 512)
            cs, csk = load_cs(qb, 512, qb % 2)

            def fnA(e, qsl=qsl, h=h):
                for m in range(4):
                    ins = e.matmul(PS[4][0:96, :], lhsT=WUQ[:, m, h * 96:(h + 1) * 96], rhs=qnT[:, m, qsl],
                                   start=(m == 0), stop=(m == 3))
                return ins
            P.op("pe", fnA, reads=[], writes=[B(4)])

            def fnB(e, qsl=qsl, h=h):
                for m in range(4):
                    ins = e.matmul(PS[5][64:96, :], lhsT=WUQR[:, m, h * 32:(h + 1) * 32], rhs=qnT[:, m, qsl],
                                   start=(m == 0), stop=(m == 3))
                return ins
            P.op("pe", fnB, reads=[], writes=[B(5)])
            P.op("act", lambda e, qsl=qsl: e.activation(out=QH[0:64, qsl], in_=PS[4][0:64, :], func=AF.Identity),
                 reads=[B(4)], writes=[("QHn", qb)])
            rope_combine(4, 5, cs, csk, 512, QH[64:96, qsl], ("QHr", qb))
        for qb in range(4):
            qsl = slice(qb * 512, (qb + 1) * 512)
            ob = 6 + (o_i[0] % 2)
            o_i[0] += 1

            def s_mms(e, p, sp, qsl=qsl):
                for i in range(2):
                    c = 2 * p + i
                    ins = e.matmul(PSP[sp][:, i * 512:(i + 1) * 512], lhsT=KT[0:96, c * 128:(c + 1) * 128], rhs=QH[0:96, qsl],
                                   start=True, stop=True)
                return ins

            def pv_mms(e, p, pt, ob=ob, VH=VH):
                for i in range(2):
                    c = 2 * p + i
                    ins = e.matmul(PS[ob][0:65, :], lhsT=VH[:, c, 0:65], rhs=T["PTP%d" % pt][:, i * 512:(i + 1) * 512],
                                   start=(c == 0), stop=(c == 65))
                return ins

            def s_keys(p):
                return [("KTn", p // 2), ("KTr", p // 2), ("QHn", qb), ("QHr", qb)]

            def rec_exp(sp, pt):
                P.op("act", lambda e, sp=sp, pt=pt: e.activation(out=T["PTP%d" % pt][:], in_=PSP[sp][:, :], func=AF.Exp, scale=ATTN_SCALE),
                     reads=[B(2 * sp), B(2 * sp + 1)], writes=[("PTP", pt)])
            slot = {}
            for p in range(3):
                sp, pt = s_i[0] % 3, pt_i[0] % 3
                s_i[0] += 1
                pt_i[0] += 1
                slot[p] = (sp, pt)
                P.op("pe", lambda e, p=p, sp=sp, s_mms=s_mms: s_mms(e, p, sp), reads=s_keys(p), writes=[B(2 * sp), B(2 * sp + 1)])
                rec_exp(sp, pt)
            for p in range(33):
                sp0, pt0 = slot.pop(p)
                if p + 3 < 33:
                    sp, pt = s_i[0] % 3, pt_i[0] % 3
                    s_i[0] += 1
                    pt_i[0] += 1
                    slot[p + 3] = (sp, pt)

                    def fn(e, p=p, pt0=pt0, sp=sp, pv_mms=pv_mms, s_mms=s_mms):
                        pv_mms(e, p, pt0)
                        return s_mms(e, p + 3, sp)
                    P.op("pe", fn, reads=[("PTP", pt0), (vk, (2 * p) // 8), ("VH1s", h % 2)] + s_keys(p + 3),
                         writes=[B(ob), B(2 * sp), B(2 * sp + 1)])
                    rec_exp(sp, pt)
                else:
                    P.op("pe", lambda e, p=p, pt0=pt0, pv_mms=pv_mms: pv_mms(e, p, pt0),
                         reads=[("PTP", pt0), (vk, (2 * p) // 8), ("VH1s", h % 2)], writes=[B(ob)])
            rden, num = T["rden"], T["num"]
            P.op("dve", lambda e, ob=ob: e.reciprocal(out=rden[64:65, :], in_=PS[ob][64:65, :]), reads=[B(ob)], writes=["rden"])
            P.op("act", lambda e, ob=ob: e.activation(out=num[:, :], in_=PS[ob][0:64, :], func=AF.Identity), reads=[B(ob)], writes=["num"])
            P.op("pe", lambda e: e.matmul(PS[5][0:64, :], lhsT=T["c1row"][64:65, 0:64], rhs=rden[64:65, :], start=True, stop=True),
                 reads=["rden", "c1row"], writes=[B(5)])
            P.op("dve", lambda e, h=h, qsl=qsl: e.tensor_tensor(out=attnT[:, h, qsl], in0=num[:, :], in1=PS[5][0:64, :], op=ALU.mult),
                 reads=["num", B(5)], writes=[("attnT", h, qb)])
    dump("attnT", attnT[:, 0, :], reads=[("attnT", 0, q) for q in range(4)])
    P.barrier()

    WOA, WOP, acc = T["WOA"], T["WOP"], T["acc"]
    dma("pool", WOA[:], w_out[0:512, :].rearrange("(h p) n -> p h n", p=64), writes=["WOA"])
    dma("pool", WOP[:], w_out[512:1024, :].rearrange("(g p) n -> p g n", p=128), writes=["WOP"])
    dma("sp", T["l1g"][:], ln1_g.partition_broadcast(128), writes=["l1g"])
    dma("sp", T["l1b"][:], ln1_b.partition_broadcast(128), writes=["l1b"])
    P.op("pool", lambda e: e.tensor_scalar(out=T["l1g"][:], in0=T["l1g"][:], scalar1=ALPHA, scalar2=None, op0=ALU.mult), reads=["l1g"], writes=["l1g"])
    P.op("pool", lambda e: e.tensor_scalar(out=T["l1b"][:], in0=T["l1b"][:], scalar1=ALPHA, scalar2=None, op0=ALU.mult), reads=["l1b"], writes=["l1b"])
    P.op("dve", lambda e: e.memset(T["stat"][:], 0.0), writes=["statz"])
    def p3_mm(t):
        tsl = slice(t * 128, (t + 1) * 128)
        for half in range(2):
            bank = (t % 2) * 2 + half

            def fn(e, tsl=tsl, half=half, bank=bank):
                for h in range(8):
                    e.matmul(PS[bank][:, :], lhsT=attnT[:, h, tsl], rhs=WOA[:, h, half * 512:(half + 1) * 512],
                             start=(h == 0), stop=False)
                for g in range(4):
                    ins = e.matmul(PS[bank][:, :], lhsT=pooledT[:, g, tsl], rhs=WOP[:, g, half * 512:(half + 1) * 512],
                                   start=False, stop=(g == 3))
                return ins
            P.op("pe", fn, reads=["WOA", "WOP"], writes=[B(bank)])
            hs = slice(half * 512, (half + 1) * 512)
            P.op("dve", lambda e, bank=bank, hs=hs, t=t: e.tensor_tensor(out=acc[:, t, hs], in0=PS[bank][:, :], in1=T["g1bc"][:, hs], op=ALU.mult),
                 reads=[B(bank)], writes=[("y", t, half)])

    def p3_ln(t):
        xs = T["xs%d" % (t % 4)]
        xsk = "xs%d" % (t % 4)
        dma("sp", xs[:], xk_t[t], writes=[xsk])
        P.op("dve", lambda e, xs=xs, t=t: e.scalar_tensor_tensor(out=xs[:], in0=xs[:], scalar=ALPHA, in1=acc[:, t, :], op0=ALU.mult, op1=ALU.add),
             reads=[xsk, ("y", t, 0), ("y", t, 1)], writes=[xsk])
        ln_tile(xs[:], acc[:, t, :], T["l1g"][:], T["l1b"][:], "st3", t, xsk, ("acc", t), extra_reads=["l1g", "l1b"])
    for t in range(17):
        if t < 16:
            p3_mm(t)
        if t >= 1:
            p3_ln(t - 1)
    dump("x1", acc[:, 0, :], reads=[("acc", 0)])
    P.barrier()

    h2T, wr = T["h2T"], T["wr"]
    dma("sp", wr[:], w_router.rearrange("(j p) n -> p j n", p=128), writes=["wr"])
    dma("sp", T["rb"][:], rbias.partition_broadcast(128), writes=["rb"])
    scores, gates = T["scores"], T["gates"]
    g2bc = T["g2bc"]
    weg = w_eg.rearrange("e (j p) f -> e p j f", p=128)
    weu = w_eu.rearrange("e (j p) f -> e p j f", p=128)
    wed = w_ed.rearrange("e (c p) d -> e p c d", p=128)

    def load_w(e_):
        i = e_ % 2
        dma("pool", T["wg%d" % i][:], weg[e_], writes=[("wg", i)])
        dma("pool", T["wu%d" % i][:], weu[e_], writes=[("wu", i)])
        dma("sp", T["wd32_%d" % i][:], wed[e_], writes=[("wd32", i)])
        P.op("pool", lambda e, i=i: e.tensor_tensor(out=T["wd%d" % i][:], in0=T["wd32_%d" % i][:], in1=bc_mid(g2bc[:], 2), op=ALU.mult),
             reads=[("wd32", i)], writes=[("wd", i)])
    load_w(0)
    def p4_front(t):
        tsl = slice(t * 128, (t + 1) * 128)
        h2f = T["h2f%d" % (t % 2)]
        pp = t % 2

        def fnT(e, t=t, pp=pp):
            for j in range(8):
                ins = e.transpose(out=PSP[pp][:, j * 128:(j + 1) * 128], in_=acc[:, t, j * 128:(j + 1) * 128], identity=idn[:])
            return ins
        P.op("pe", fnT, reads=["ident", ("acc", t)], writes=[B(2 * pp), B(2 * pp + 1)])
        for j in range(8):
            P.op("act", lambda e, j=j, pp=pp, h2f=h2f: e.activation(out=h2f[:, j, :], in_=PSP[pp][:, j * 128:(j + 1) * 128], func=AF.Identity,
                                                                    bias=modv[:, 24 + j, 0:1], scale=mods[:, 16 + j:16 + j + 1]),
                 reads=[B(2 * pp), B(2 * pp + 1)], writes=[("h2f", t % 2, j)])
        P.op("dve", lambda e, tsl=tsl, h2f=h2f: e.tensor_copy(out=h2T[:, :, tsl], in_=h2f[:, :, :]),
             reads=[("h2f", t % 2, j) for j in range(8)], writes=[("h2T", t)])

    def p4_back(t):
        h2f = T["h2f%d" % (t % 2)]

        def fn(e, t=t, h2f=h2f):
            for j in range(8):
                ins = e.matmul(PS[4 + t % 2][:, 0:NEXP], lhsT=h2f[:, j, :], rhs=wr[:, j, :], start=(j == 0), stop=(j == 7))
            return ins
        P.op("pe", fn, reads=["wr"] + [("h2f", t % 2, j) for j in range(8)], writes=[B(4 + t % 2)])
        P.op("act", lambda e, t=t: e.activation(out=scores[:, t, :], in_=PS[4 + t % 2][:, 0:NEXP], func=AF.Sigmoid),
             reads=[B(4 + t % 2)], writes=[("scores", t)])
    for t in range(17):
        if t < 16:
            p4_front(t)
        if t >= 1:
            p4_back(t - 1)
    sk = [("scores", t) for t in range(16)]
    ra, rbb, rc = T["ra"], T["rbb"], T["rc"]
    rs, rs2, rs3, rs4, t8 = T["rs"], T["rs2"], T["rs3"], T["rs4"], T["t8"]
    v4 = lambda a: a[:].rearrange("p t (g k) -> p (t g) k", k=8)
    f2 = lambda a: a[:].rearrange("p t g -> p (t g)")
    P.op("dve", lambda e: e.tensor_tensor(out=ra[:], in0=scores[:], in1=bc_mid(T["rb"][:], 16), op=ALU.add),
         reads=sk + ["rb"], writes=["ra"])
    P.op("dve", lambda e: e.tensor_reduce(out=f2(rs), in_=v4(ra), axis=AX.X, op=ALU.max), reads=["ra"], writes=["rs"])
    P.op("dve", lambda e: e.tensor_tensor(out=v4(rbb), in0=v4(ra), in1=bc_last(f2(rs), 8), op=ALU.is_equal),
         reads=["ra", "rs"], writes=["rbb"])
    P.op("dve", lambda e: e.scalar_tensor_tensor(out=rbb[:], in0=rbb[:], scalar=-10.0, in1=ra[:], op0=ALU.mult, op1=ALU.add),
         reads=["rbb", "ra"], writes=["rbb"])
    P.op("dve", lambda e: e.tensor_reduce(out=f2(rs2), in_=v4(rbb), axis=AX.X, op=ALU.max), reads=["rbb"], writes=["rs2"])
    P.op("dve", lambda e: e.tensor_tensor(out=rs[:], in0=rs[:], in1=rs2[:], op=ALU.add), reads=["rs", "rs2"], writes=["rs"])
    for t in range(16):
        P.op("dve", lambda e, t=t: e.max(out=t8[:, t, :], in_=rs[:, t, :]), reads=["rs"], writes=[("t8", t)])
    t8k = [("t8", t) for t in range(16)]
    P.op("dve", lambda e: e.tensor_tensor(out=rs3[:], in0=rs[:], in1=bc_last(t8[:, :, 3], 8), op=ALU.is_ge),
         reads=["rs"] + t8k, writes=["rs3"])
    P.op("dve", lambda e: e.tensor_scalar(out=rs4[:], in0=rs3[:], scalar1=10.0, scalar2=-10.0, op0=ALU.mult, op1=ALU.add),
         reads=["rs3"], writes=["rs4"])
    P.op("dve", lambda e: e.tensor_tensor(out=v4(rbb), in0=v4(ra), in1=bc_last(f2(rs3), 8), op=ALU.mult),
         reads=["ra", "rs3", "rs2"], writes=["rbb"])
    P.op("dve", lambda e: e.tensor_tensor(out=v4(rbb), in0=v4(rbb), in1=bc_last(f2(rs4), 8), op=ALU.add),
         reads=["rbb", "rs4"], writes=["rbb"])
    for t in range(16):
        P.op("dve", lambda e, t=t: e.max(out=t8[:, t, :], in_=rbb[:, t, :]), reads=["rbb", "rs3"], writes=[("t8", t)])
    P.op("dve", lambda e: e.tensor_tensor(out=rc[:], in0=rbb[:], in1=bc_last(t8[:, :, 7], NEXP), op=ALU.is_ge),
         reads=["rbb"] + t8k, writes=["rc"])
    P.op("dve", lambda e: e.tensor_tensor(out=rc[:], in0=rc[:], in1=scores[:], op=ALU.mult), reads=["rc"] + sk, writes=["rc"])
    P.op("dve", lambda e: e.tensor_reduce(out=T["rsum"][:], in_=rc[:], axis=AX.X, op=ALU.add), reads=["rc"], writes=["rsum"])
    P.op("dve", lambda e: e.reciprocal(out=T["rsum"][:], in_=T["rsum"][:]), reads=["rsum"], writes=["rsum"])
    P.op("dve", lambda e: e.scalar_tensor_tensor(out=gates[:], in0=rc[:], scalar=2.5, in1=bc_last(T["rsum"][:], NEXP),
                                                 op0=ALU.mult, op1=ALU.mult),
         reads=["rc", "rsum"], writes=["gates"])
    dump("gates", gates[:, 0, :], reads=["gates"])
    P.barrier()

    y_i = [0]
    u_i = [0]

    def gu_half(e_, blk, u, c):
        i = e_ % 2
        tsl = slice(blk * 512, (blk + 1) * 512)
        aT = T["aT%d" % u]
        bg, bu = c * 2, c * 2 + 1

        def fn(e):
            for j in range(8):
                e.matmul(PS[bg][:, :], lhsT=T["wg%d" % i][:, j, c * 128:(c + 1) * 128], rhs=h2T[:, j, tsl], start=(j == 0), stop=(j == 7))
            for j in range(8):
                ins = e.matmul(PS[bu][:, :], lhsT=T["wu%d" % i][:, j, c * 128:(c + 1) * 128], rhs=h2T[:, j, tsl], start=(j == 0), stop=(j == 7))
            return ins
        P.op("pe", fn, reads=[("wg", i), ("wu", i)], writes=[B(bg), B(bu)])
        sg = T["sg%d" % c]
        P.op("act", lambda e: e.activation(out=sg[:], in_=PS[bg][:, :], func=AF.Silu), reads=[B(bg)], writes=[("sg", c)])
        P.op("dve", lambda e: e.tensor_tensor(out=aT[:, c, :], in0=sg[:], in1=PS[bu][:, :], op=ALU.mult),
             reads=[("sg", c), B(bu)], writes=[("aT", u, c)])

    def down_half(e_, blk, u, hh):
        i = e_ % 2
        aT = T["aT%d" % u]

        def fn(e):
            for k in range(2):
                tt = hh * 2 + k
                for half in range(2):
                    bank = 4 + k * 2 + half
                    for c in range(2):
                        ins = e.matmul(PS[bank][:, :], lhsT=aT[:, c, tt * 128:(tt + 1) * 128],
                                       rhs=T["wd%d" % i][:, c, half * 512:(half + 1) * 512], start=(c == 0), stop=(c == 1))
            return ins
        P.op("pe", fn, reads=[("aT", u, 0), ("aT", u, 1), ("wd", i)], writes=[B(4), B(5), B(6), B(7)])
        for k in range(2):
            t = blk * 4 + hh * 2 + k
            for half in range(2):
                bank = 4 + k * 2 + half
                hs = slice(half * 512, (half + 1) * 512)
                if e_ < NEXP:
                    P.op("dve", lambda e, t=t, hs=hs, bank=bank: e.scalar_tensor_tensor(
                        out=acc[:, t, hs], in0=PS[bank][:, :], scalar=gates[:, t, e_:e_ + 1], in1=acc[:, t, hs],
                        op0=ALU.mult, op1=ALU.add),
                        reads=[B(bank)], writes=[("acc", t, half)])
                else:
                    P.op("dve", lambda e, t=t, hs=hs, bank=bank: e.tensor_tensor(
                        out=acc[:, t, hs], in0=PS[bank][:, :], in1=acc[:, t, hs], op=ALU.add),
                        reads=[B(bank)], writes=[("acc", t, half)])

    dma("sp", T["l2g"][:], ln2_g.partition_broadcast(128), writes=["l2g"])
    dma("sp", T["l2b"][:], ln2_b.partition_broadcast(128), writes=["l2b"])
    prev = None
    u_i = [0]
    for e_ in range(NEXP + 1):
        for blk in range(4):
            u = u_i[0] % 2
            u_i[0] += 1
            gu_half(e_, blk, u, 0)
            if prev is not None:
                down_half(*prev, 0)
            gu_half(e_, blk, u, 1)
            if prev is not None:
                down_half(*prev, 1)
            prev = (e_, blk, u)
            if blk == 0 and e_ + 1 <= NEXP:
                load_w(e_ + 1)
    down_half(*prev, 0)
    down_half(*prev, 1)

    finals = []
    P.op("dve", lambda e: e.memset(T["stat"][:], 0.0), writes=["statz"])
    for t in range(16):
        o = T["o%d" % (t % 4)]
        ok = "o%d" % (t % 4)
        ln_tile(acc[:, t, :], o[:], T["l2g"][:], T["l2b"][:], "st6", t, ("acc", t, 0), ok, extra_reads=["l2g", "l2b", ("acc", t, 1)])
        finals.append(dma("sp", yout[t * 128:(t + 1) * 128, :], o[:], reads=[ok], writes=[("yout", t % 4)]))
    P.op("sp", None, extra_deps=finals + [i for i, o in enumerate(P.ops) if o["dma"] and isinstance(o["chan"], tuple) and o["chan"][0] == "dbg"])

    P.finalize(nc, st)
    with nc.Block() as block:
        @block.sync
        def _(e):
            P.emit("sp", e)

        @block.scalar
        def _(e):
            P.emit("act", e)

        @block.vector
        def _(e):
            P.emit("dve", e)

        @block.gpsimd
        def _(e):
            P.emit("pool", e)

        @block.tensor
        def _(e):
            P.emit("pe", e)
    st.close()
    return nc, P


def _rope_tables(own0):
    t_own = np.arange(own0, own0 + TOWN)
    t_rest = np.concatenate([np.arange(0, own0), np.arange(own0 + TOWN, SEQ)])
    t = np.concatenate([t_own, t_rest]).astype(np.int64)
    pos = np.stack([t // 64, t % 64], axis=0).astype(np.float32)
    inv = (np.float32(10000.0) ** (-np.arange(8, dtype=np.float32) / np.float32(8.0))).astype(np.float32)
    ang = pos[:, None, :] * inv[None, :, None]
    cos = np.cos(ang).astype(np.float32)
    sin = np.sin(ang).astype(np.float32)
    cos32 = np.stack([cos[a, f] for a in range(2) for hf in range(2) for f in range(8)], 0)
    sin32 = np.stack([sin[a, f] for a in range(2) for hf in range(2) for f in range(8)], 0)
    cosk = np.concatenate([cos32, np.ones((32, CTX), np.float32)], 1)
    sink = np.concatenate([sin32, np.zeros((32, CTX), np.float32)], 1)
    return np.ascontiguousarray(cosk), np.ascontiguousarray(sink)


def make_in_maps(x, c, ctx, c_ctx, w_ada, b_ada, w_in, q_norm_g, w_uq, kv_norm_g, w_ukv,
                 w_pool, pool_scale, w_out, ln1_g, ln1_b, w_router, router_bias,
                 w_e_gate, w_e_up, w_e_down, w_s_gate, w_s_up, w_s_down, ln2_g, ln2_b):
    f = lambda a: np.ascontiguousarray(np.asarray(a, dtype=np.float32))
    x, c, ctx, c_ctx = f(x), f(c), f(ctx), f(c_ctx)
    fm = lambda v, n: np.ascontiguousarray(f(v).reshape(n, 128).T)
    shared = dict(
        ident=np.eye(128, dtype=np.float32),
        w_ada=f(w_ada[0]), b_ada_fm=fm(b_ada[0], 48), b_ada_row=f(b_ada[0]).reshape(1, -1),
        w_in=f(w_in[0]), qg_fm=fm(q_norm_g[0], 4), kvg_fm=fm(kv_norm_g[0], 2),
        w_uq=f(w_uq[0]), w_ukv=f(w_ukv[0]), w_pool=f(w_pool[0]), pscale_fm=fm(pool_scale[0], 4),
        w_out=f(w_out[0]), ln1_g=f(ln1_g[0]).reshape(1, -1), ln1_b=f(ln1_b[0]).reshape(1, -1),
        ln2_g=f(ln2_g[0]).reshape(1, -1), ln2_b=f(ln2_b[0]).reshape(1, -1),
        w_router=f(w_router[0]), rbias=f(router_bias[0]).reshape(1, -1),
        w_eg=np.concatenate([f(w_e_gate[0]), f(w_s_gate[0])[None]], 0),
        w_eu=np.concatenate([f(w_e_up[0]), f(w_s_up[0])[None]], 0),
        w_ed=np.concatenate([f(w_e_down[0]), f(w_s_down[0])[None]], 0),
    )
    maps = []
    for core in range(NCORE):
        b, own0 = core // 4, (core % 4) * TOWN
        xb = x[b]
        xk = np.concatenate([xb[own0:own0 + TOWN], xb[:own0], xb[own0 + TOWN:], ctx[b]], 0)
        xh = np.zeros((4, 16, D), np.float32)
        hm = np.zeros((128, 4, 16), np.float32)
        for blk in range(4):
            s0 = own0 + blk * 512
            for k in range(8):
                tl, tr = s0 - 8 + k, s0 + 512 + k
                if 0 <= tl < SEQ:
                    xh[blk, k] = xb[tl]
                    hm[:, blk, k] = 1.0
                if 0 <= tr < SEQ:
                    xh[blk, 8 + k] = xb[tr]
                    hm[:, blk, 8 + k] = 1.0
        ic = np.zeros((16, 512), np.float32)
        for blk in range(4):
            tok = own0 + blk * 512 + np.arange(512)
            for g in range(4):
                w = 2 << g
                lo = np.clip(tok - w // 2, 0, SEQ)
                hi = np.clip(tok - w // 2 + w, 0, SEQ)
                ic[blk * 4 + g] = (np.float32(1.0) / (hi - lo).astype(np.float32))
        cosk, sink = _rope_tables(own0)
        cv = np.stack([fm(c[b], 8), fm(c_ctx, 8)], axis=-1)
        m = dict(shared)
        m.update(xk=np.ascontiguousarray(xk), xhalo=xh, hmask=hm, cosk=cosk, sink=sink, cvec=np.ascontiguousarray(cv), icntc=ic)
        maps.append(m)
    return maps


_CACHE = {}


def kernel(**inputs):
    if "nc" not in _CACHE:
        _CACHE["nc"] = build_program()[0]
    nc = _CACHE["nc"]
    in_maps = make_in_maps(**inputs)
    res = run_bass_kernel_spmd(nc, in_maps, core_ids=list(range(NCORE)))
    out = np.zeros((2, SEQ, D), np.float32)
    for core in range(NCORE):
        b, own0 = core // 4, (core % 4) * TOWN
        out[b, own0:own0 + TOWN] = res.results[core]["yout"]
    return out
```
